# Optimizing a Trainium2 kernel written in Bass

```python
import numpy as np
import jax, jax.numpy as jnp
from jax import lax

D_MODEL = 1024
BATCH = 2
SEQ = 8192
DEPTH = 4

HEAD_DIM = 64
NSA_HEADS = 6
NSA_KV_HEADS = 2
NSA_GROUP = NSA_HEADS // NSA_KV_HEADS
RWKV_HEADS = 4
MOBA_HEADS = 6
D_NSA = NSA_HEADS * HEAD_DIM
D_NSA_KV = NSA_KV_HEADS * HEAD_DIM
D_RWKV = RWKV_HEADS * HEAD_DIM
D_MOBA = MOBA_HEADS * HEAD_DIM
D_MIX = D_NSA + D_RWKV + D_MOBA

CMP_LEN = 32
CMP_STRIDE = 16
CMP_HIDDEN = 256
SLC_BLOCK = 64
SLC_TOPK = 16
WINDOW = 512
N_BRANCH = 3
RW_DECAY_LORA = 32
RW_AAA_LORA = 32
RW_GATE_LORA = 64
RW_DECAY_SCALE = 0.606531
RW_LN_EPS = 64e-5
MOBA_BLOCK = 256
MOBA_TOPK = 3
D_FF = 2816
CONV_WIDTH = 3
Q_CHUNK = 128
NORM_EPS = 1e-6
BIG = 1e9

N_IN_NSA = D_NSA + 6 * D_NSA_KV + NSA_HEADS * N_BRANCH
N_IN_RWKV = 3 * D_RWKV + RW_DECAY_LORA + RW_AAA_LORA + RW_GATE_LORA
N_IN_MOBA = 3 * D_MOBA
N_IN = N_IN_NSA + N_IN_RWKV + N_IN_MOBA

kernel_name = 'hymba_nsa_rwkv7_moba_convglu'


def _rmsnorm(x, g):
    xf = x.astype(jnp.float32)
    y = xf * lax.rsqrt(jnp.mean(xf * xf, axis=-1, keepdims=True) + NORM_EPS)
    return (y * g.astype(jnp.float32)).astype(x.dtype)


def _head_rmsnorm(o, g):
    b, s, d = o.shape
    oh = o.reshape(b, s, d // HEAD_DIM, HEAD_DIM)
    return _rmsnorm(oh, g.reshape(d // HEAD_DIM, HEAD_DIM)).reshape(b, s, d)


def _masked_softmax(s, mask):
    s = jnp.where(mask, s.astype(jnp.float32), -jnp.inf)
    m = jnp.max(s, axis=-1, keepdims=True)
    m = jnp.where(jnp.isfinite(m), m, 0.0)
    p = jnp.exp(s - m)
    return p / jnp.maximum(jnp.sum(p, axis=-1, keepdims=True), 1e-30)


def _split(a, sizes):
    return jnp.split(a, [int(i) for i in np.cumsum(sizes)[:-1]], axis=-1)


def _nsa_compress(kv, pos, w1, w2):
    b, s, g, d = kv.shape
    n_cmp = (s - CMP_LEN) // CMP_STRIDE + 1
    idx = np.arange(n_cmp)[:, None] * CMP_STRIDE + np.arange(CMP_LEN)[None, :]
    blk = kv[:, idx] + pos[:, None, :]
    blk = jnp.transpose(blk, (0, 3, 1, 2, 4)).reshape(b, g, n_cmp, CMP_LEN * d)
    return jax.nn.gelu(blk @ w1) @ w2


def _cmp_to_slc(n_cmp, n_slc):
    r = SLC_BLOCK // CMP_STRIDE
    c = CMP_LEN // CMP_STRIDE
    i = (r * np.arange(n_slc)[:, None, None] - np.arange(r)[None, :, None] - np.arange(c)[None, None, :]).reshape(n_slc, -1)
    m = (i[:, :, None] == np.arange(n_cmp)[None, None, :]).sum(1)
    return jnp.asarray(m.T, jnp.float32)


def _nsa_mixer(p, cmp_pos, cmp_w1, cmp_w2):
    b, s, _ = p.shape
    q, kc, vc, ks, vs, kw, vw, gt = _split(p, [D_NSA] + [D_NSA_KV] * 6 + [NSA_HEADS * N_BRANCH])
    kvh = lambda t: t.reshape(b, s, NSA_KV_HEADS, HEAD_DIM)
    hf = lambda t: jnp.transpose(kvh(t), (0, 2, 1, 3))
    q = jnp.transpose(q.reshape(b, s, NSA_KV_HEADS, NSA_GROUP, HEAD_DIM), (0, 2, 3, 1, 4))
    gt = jnp.transpose(jax.nn.sigmoid(gt.reshape(b, s, NSA_KV_HEADS, NSA_GROUP, N_BRANCH)), (0, 2, 3, 1, 4))
    kc = _nsa_compress(kvh(kc), cmp_pos[0], cmp_w1[0], cmp_w2[0])
    vc = _nsa_compress(kvh(vc), cmp_pos[1], cmp_w1[1], cmp_w2[1])
    n_cmp = kc.shape[2]
    n_slc = s // SLC_BLOCK
    k_slc = min(SLC_TOPK, n_slc)
    ks = hf(ks).reshape(b, NSA_KV_HEADS, n_slc, SLC_BLOCK, HEAD_DIM)
    vs = hf(vs).reshape(b, NSA_KV_HEADS, n_slc, SLC_BLOCK, HEAD_DIM)
    kw = jnp.pad(hf(kw), ((0, 0), (0, 0), (WINDOW, 0), (0, 0)))
    vw = jnp.pad(hf(vw), ((0, 0), (0, 0), (WINDOW, 0), (0, 0)))
    cmp_map = _cmp_to_slc(n_cmp, n_slc)
    cmp_end = jnp.asarray(np.arange(n_cmp) * CMP_STRIDE + CMP_LEN - 1)
    blk_id = jnp.arange(n_slc)
    bi = jnp.arange(b)[:, None, None, None]
    gi = jnp.arange(NSA_KV_HEADS)[None, :, None, None]
    scale = HEAD_DIM ** -0.5

    def chunk(c):
        t0 = c * Q_CHUNK
        tpos = t0 + jnp.arange(Q_CHUNK)
        qc = lax.dynamic_slice_in_dim(q, t0, Q_CHUNK, axis=3)
        gc = lax.dynamic_slice_in_dim(gt, t0, Q_CHUNK, axis=3)
        pc = _masked_softmax(jnp.einsum('bgrqd,bgnd->bgrqn', qc, kc) * scale, cmp_end[None, :] <= tpos[:, None])
        oc = jnp.einsum('bgrqn,bgnd->bgrqd', pc.astype(vc.dtype), vc)
        imp = jnp.einsum('bgrqn,nj->bgqj', pc, cmp_map)
        cur = tpos // SLC_BLOCK
        forced = (blk_id[None, :] == 0) | (blk_id[None, :] == cur[:, None]) | (blk_id[None, :] == cur[:, None] - 1)
        causal = blk_id[None, :] * SLC_BLOCK <= tpos[:, None]
        imp = jnp.where(forced, BIG, jnp.where(causal, imp, -BIG))
        _, sel = lax.top_k(imp, k_slc)
        kg = ks[bi, gi, sel].reshape(b, NSA_KV_HEADS, Q_CHUNK, k_slc * SLC_BLOCK, HEAD_DIM)
        vg = vs[bi, gi, sel].reshape(b, NSA_KV_HEADS, Q_CHUNK, k_slc * SLC_BLOCK, HEAD_DIM)
        kpos = (sel[..., None] * SLC_BLOCK + jnp.arange(SLC_BLOCK)).reshape(b, NSA_KV_HEADS, Q_CHUNK, k_slc * SLC_BLOCK)
        ps = _masked_softmax(jnp.einsum('bgrqd,bgqkd->bgrqk', qc, kg) * scale, (kpos <= tpos[:, None])[:, :, None])
        osl = jnp.einsum('bgrqk,bgqkd->bgrqd', ps.astype(vg.dtype), vg)
        kwc = lax.dynamic_slice_in_dim(kw, t0, Q_CHUNK + WINDOW, axis=2)
        vwc = lax.dynamic_slice_in_dim(vw, t0, Q_CHUNK + WINDOW, axis=2)
        wpos = t0 - WINDOW + jnp.arange(Q_CHUNK + WINDOW)
        dist = tpos[:, None] - wpos[None, :]
        mw = (dist >= 0) & (dist < WINDOW) & (wpos[None, :] >= 0)
        pw = _masked_softmax(jnp.einsum('bgrqd,bgkd->bgrqk', qc, kwc) * scale, mw)
        ow = jnp.einsum('bgrqk,bgkd->bgrqd', pw.astype(vwc.dtype), vwc)
        return gc[..., 0:1] * oc + gc[..., 1:2] * osl + gc[..., 2:3] * ow

    o = lax.map(chunk, jnp.arange(s // Q_CHUNK))
    return jnp.transpose(o, (1, 0, 4, 2, 3, 5)).reshape(b, s, D_NSA)


def _rwkv7_mixer(p, mu, w0, w2, a0, a2, g2, k_k, k_a, r_k, lnx_w, lnx_b):
    b, s, _ = p.shape
    f32 = jnp.float32
    p_prev = jnp.pad(p, ((0, 0), (1, 0), (0, 0)))[:, :-1]
    p = p + (p_prev - p) * mu
    r, k, v, xw, xa, xg = _split(p, [D_RWKV] * 3 + [RW_DECAY_LORA, RW_AAA_LORA, RW_GATE_LORA])
    w = jnp.exp(-RW_DECAY_SCALE * jax.nn.sigmoid((w0 + jnp.tanh(xw) @ w2).astype(f32)))
    a = jax.nn.sigmoid(a0 + xa @ a2)
    g = jax.nn.sigmoid(xg) @ g2
    heads = lambda t: t.reshape(b, s, RWKV_HEADS, HEAD_DIM).astype(f32)
    kk = heads(k * k_k)
    kk = kk / jnp.maximum(jnp.sqrt(jnp.sum(kk * kk, axis=-1, keepdims=True)), 1e-12)
    k = k * (1.0 + (a - 1.0) * k_a)
    r_h, w_h, k_h, v_h, a_h = heads(r), heads(w), heads(k), heads(v), heads(a)

    def step(state, inp):
        r_t, w_t, k_t, v_t, kk_t, a_t = inp
        sa = jnp.einsum('bhvk,bhk->bhv', state, -kk_t)
        state = state * w_t[:, :, None, :] + sa[..., None] * (kk_t * a_t)[:, :, None, :] + v_t[..., None] * k_t[:, :, None, :]
        return state, jnp.einsum('bhvk,bhk->bhv', state, r_t)

    xs = tuple(jnp.moveaxis(t, 1, 0) for t in (r_h, w_h, k_h, v_h, kk, a_h))
    _, out = lax.scan(step, jnp.zeros((b, RWKV_HEADS, HEAD_DIM, HEAD_DIM), f32), xs)
    out = jnp.moveaxis(out, 0, 1)
    mean = jnp.mean(out, axis=-1, keepdims=True)
    var = jnp.mean(jnp.square(out - mean), axis=-1, keepdims=True)
    out = ((out - mean) * lax.rsqrt(var + RW_LN_EPS)).reshape(b, s, D_RWKV) * lnx_w + lnx_b
    bonus = jnp.sum(r_h * k_h * r_k, axis=-1, keepdims=True) * v_h
    out = (out + bonus.reshape(b, s, D_RWKV)) * g
    return out.astype(p.dtype)


def _moba_mixer(p):
    b, s, _ = p.shape
    q, k, v = [jnp.transpose(t.reshape(b, s, MOBA_HEADS, HEAD_DIM), (0, 2, 1, 3)) for t in _split(p, [D_MOBA] * 3)]
    n_blk = -(-s // MOBA_BLOCK)
    pad = n_blk * MOBA_BLOCK - s
    k = jnp.pad(k, ((0, 0), (0, 0), (0, pad), (0, 0)))
    v = jnp.pad(v, ((0, 0), (0, 0), (0, pad), (0, 0)))
    kb = k.reshape(b, MOBA_HEADS, n_blk, MOBA_BLOCK, HEAD_DIM)
    vb = v.reshape(b, MOBA_HEADS, n_blk, MOBA_BLOCK, HEAD_DIM)
    kmean = jnp.mean(kb.astype(jnp.float32), axis=3)
    n_sel = min(MOBA_TOPK, max(n_blk - 1, 1))
    n_g = n_sel * MOBA_BLOCK
    blk_id = jnp.arange(n_blk)
    bi = jnp.arange(b)[:, None, None, None]
    hi = jnp.arange(MOBA_HEADS)[None, :, None, None]
    scale = HEAD_DIM ** -0.5

    def chunk(c):
        t0 = c * Q_CHUNK
        tpos = t0 + jnp.arange(Q_CHUNK)
        cur = t0 // MOBA_BLOCK
        qc = lax.dynamic_slice_in_dim(q, t0, Q_CHUNK, axis=2)
        gate = jnp.einsum('bhqd,bhnd->bhqn', qc.astype(jnp.float32), kmean)
        gate = jnp.where(blk_id < cur, gate, -jnp.inf)
        _, sel = lax.top_k(gate, n_sel)
        kg = kb[bi, hi, sel].reshape(b, MOBA_HEADS, Q_CHUNK, n_g, HEAD_DIM)
        vg = vb[bi, hi, sel].reshape(b, MOBA_HEADS, Q_CHUNK, n_g, HEAD_DIM)
        mg = jnp.repeat(sel < cur, MOBA_BLOCK, axis=-1)
        ko = lax.dynamic_slice_in_dim(k, cur * MOBA_BLOCK, MOBA_BLOCK, axis=2)
        vo = lax.dynamic_slice_in_dim(v, cur * MOBA_BLOCK, MOBA_BLOCK, axis=2)
        mo = (cur * MOBA_BLOCK + jnp.arange(MOBA_BLOCK))[None, :] <= tpos[:, None]
        s_g = jnp.einsum('bhqd,bhqkd->bhqk', qc, kg)
        s_o = jnp.einsum('bhqd,bhkd->bhqk', qc, ko)
        mask = jnp.concatenate([mg, jnp.broadcast_to(mo, s_o.shape)], axis=-1)
        prob = _masked_softmax(jnp.concatenate([s_g, s_o], axis=-1) * scale, mask).astype(v.dtype)
        return jnp.einsum('bhqk,bhqkd->bhqd', prob[..., :n_g], vg) + jnp.einsum('bhqk,bhkd->bhqd', prob[..., n_g:], vo)

    o = lax.map(chunk, jnp.arange(s // Q_CHUNK))
    return jnp.transpose(o, (1, 0, 3, 2, 4)).reshape(b, s, D_MOBA)


def _conv_glu(h, w_in, conv_w, conv_b, w_out):
    u, gt = jnp.split(h @ w_in, 2, axis=-1)
    gt = lax.conv_general_dilated(gt, conv_w[:, None, :], window_strides=(1,), padding=[(CONV_WIDTH - 1, 0)], dimension_numbers=('NWC', 'WIO', 'NWC'), feature_group_count=D_FF) + conv_b
    return (jax.nn.silu(gt) * u) @ w_out


def setup_inputs(seed: int = 0) -> dict:
    key = jax.random.key(seed)
    ks = iter(jax.random.split(key, 32))
    nrm = lambda shape, sc: jax.random.normal(next(ks), shape, jnp.float32) * sc
    gain = lambda shape: 1.0 + nrm(shape, 0.02)
    L = DEPTH
    resid = (2 * DEPTH) ** -0.5
    return {
        'x': nrm((BATCH, SEQ, D_MODEL), 1.0),
        'attn_norm': gain((L, D_MODEL)),
        'w_in': nrm((L, D_MODEL, N_IN), D_MODEL ** -0.5),
        'nsa_cmp_pos': nrm((L, 2, CMP_LEN, HEAD_DIM), 0.1),
        'nsa_cmp_w1': nrm((L, 2, CMP_LEN * HEAD_DIM, CMP_HIDDEN), (CMP_LEN * HEAD_DIM) ** -0.5),
        'nsa_cmp_w2': nrm((L, 2, CMP_HIDDEN, HEAD_DIM), CMP_HIDDEN ** -0.5),
        'nsa_out_gain': gain((L, D_NSA)),
        'rw_mu': jax.random.uniform(next(ks), (L, N_IN_RWKV), jnp.float32),
        'rw_w0': nrm((L, D_RWKV), 0.5),
        'rw_w2': nrm((L, RW_DECAY_LORA, D_RWKV), 0.5 * RW_DECAY_LORA ** -0.5),
        'rw_a0': nrm((L, D_RWKV), 0.5),
        'rw_a2': nrm((L, RW_AAA_LORA, D_RWKV), 0.5 * RW_AAA_LORA ** -0.5),
        'rw_g2': nrm((L, RW_GATE_LORA, D_RWKV), RW_GATE_LORA ** -0.5),
        'rw_k_k': 1.0 + nrm((L, D_RWKV), 0.1),
        'rw_k_a': 1.0 + nrm((L, D_RWKV), 0.1),
        'rw_r_k': nrm((L, RWKV_HEADS, HEAD_DIM), 0.1),
        'rw_lnx_w': gain((L, D_RWKV)),
        'rw_lnx_b': nrm((L, D_RWKV), 0.01),
        'moba_out_gain': gain((L, D_MOBA)),
        'w_out': nrm((L, D_MIX, D_MODEL), D_MIX ** -0.5 * resid),
        'ffn_norm': gain((L, D_MODEL)),
        'ffn_w_in': nrm((L, D_MODEL, 2 * D_FF), D_MODEL ** -0.5),
        'ffn_conv_w': nrm((L, CONV_WIDTH, D_FF), CONV_WIDTH ** -0.5),
        'ffn_conv_b': nrm((L, D_FF), 0.01),
        'ffn_w_out': nrm((L, D_FF, D_MODEL), D_FF ** -0.5 * resid),
        'final_norm': gain((D_MODEL,)),
    }


def reference(x, attn_norm, w_in, nsa_cmp_pos, nsa_cmp_w1, nsa_cmp_w2, nsa_out_gain, rw_mu, rw_w0, rw_w2, rw_a0, rw_a2, rw_g2, rw_k_k, rw_k_a, rw_r_k, rw_lnx_w, rw_lnx_b, moba_out_gain, w_out, ffn_norm, ffn_w_in, ffn_conv_w, ffn_conv_b, ffn_w_out, final_norm):
    for l in range(DEPTH):
        h = _rmsnorm(x, attn_norm[l])
        proj = h @ w_in[l]
        p_nsa, p_rw, p_moba = _split(proj, [N_IN_NSA, N_IN_RWKV, N_IN_MOBA])
        o_nsa = _head_rmsnorm(_nsa_mixer(p_nsa, nsa_cmp_pos[l], nsa_cmp_w1[l], nsa_cmp_w2[l]), nsa_out_gain[l])
        o_rw = _rwkv7_mixer(p_rw, rw_mu[l], rw_w0[l], rw_w2[l], rw_a0[l], rw_a2[l], rw_g2[l], rw_k_k[l], rw_k_a[l], rw_r_k[l], rw_lnx_w[l], rw_lnx_b[l])
        o_moba = _head_rmsnorm(_moba_mixer(p_moba), moba_out_gain[l])
        x = x + jnp.concatenate([o_nsa, o_rw, o_moba], axis=-1) @ w_out[l]
        x = x + _conv_glu(_rmsnorm(x, ffn_norm[l]), ffn_w_in[l], ffn_conv_w[l], ffn_conv_b[l], ffn_w_out[l])
    return _rmsnorm(x, final_norm)
```

```python
from contextlib import ExitStack
import numpy as np
import concourse.bass as bass
import concourse.mybir as mybir
from concourse.bass_utils import run_bass_kernel_spmd

F32 = mybir.dt.float32
BF16 = mybir.dt.bfloat16
AF = mybir.ActivationFunctionType
ALU = mybir.AluOpType
AX = mybir.AxisListType

D_MODEL = 1024
BATCH = 2
SEQ = 8192
DEPTH = 4
HD = 64
N_IN = 3218
N_IN_NSA = 1170
N_IN_RWKV = 896
N_IN_MOBA = 1152
D_FF = 2816
NORM_EPS = 1e-6
NCORES = 8
NEG = -1000.0


class Buf:
    __slots__ = ("name", "last_w", "readers", "dsem", "dcount")

    def __init__(self, name):
        self.name = name
        self.last_w = None
        self.readers = {}
        self.dsem = None
        self.dcount = 0


class Prog:
    ENGS = ("sync", "gpsimd", "scalar", "vector", "tensor")
    NDSEM = 48

    def __init__(self, nc, es):
        self.nc = nc
        self.es = es
        self.lists = {e: [] for e in self.ENGS}
        self.count = {e: 0 for e in self.ENGS}
        self.sem = {e: es.enter_context(nc.semaphore("s_" + e)) for e in self.ENGS}
        self.waited = {e: {} for e in self.ENGS}
        self.pool = [[es.enter_context(nc.semaphore("d%d" % i)), 0] for i in range(self.NDSEM)]
        self.free = list(range(self.NDSEM))
        self.stage_bufs = []
        self.stores = {}
        self.ninst = 0

    def buf(self, name):
        return Buf(name)

    def _dsem(self, b):
        if b.dsem is None:
            i = self.free.pop()
            b.dsem = self.pool[i][0]
            b.dcount = self.pool[i][1]
            self.stage_bufs.append((b, i))
        return b.dsem

    def _need(self, eng, deps):
        w = self.waited[eng]
        best = {}
        for sem, val in deps:
            k = id(sem)
            if w.get(k, 0) >= val:
                continue
            if k not in best or best[k][1] < val:
                best[k] = (sem, val)
        for k, (sem, val) in best.items():
            self.lists[eng].append(("wait", sem, val))
            w[k] = val

    @staticmethod
    def _deps(reads, writes):
        deps = []
        for b in reads:
            if b.last_w is not None:
                deps.append(b.last_w)
        for b in writes:
            if b.last_w is not None:
                deps.append(b.last_w)
            deps.extend(b.readers.values())
        return deps

    @staticmethod
    def _mark(tick, reads, writes):
        k = id(tick[0])
        for b in reads:
            if k not in b.readers or b.readers[k][1] < tick[1]:
                b.readers[k] = tick
        for b in writes:
            b.last_w = tick
            b.readers = {}

    def op(self, eng, fn, reads=(), writes=(), inc=True):
        deps = self._deps(reads, writes)
        own = self.sem[eng]
        if eng == "tensor":
            deps = [d for d in deps if d[0] is not own]
        self._need(eng, deps)
        self.ninst += 1
        if inc:
            self.count[eng] += 1
            tick = (own, self.count[eng])
            self.lists[eng].append(("op", fn, own, 1))
        else:
            assert eng == "tensor"
            tick = (own, self.count[eng] + 1)
            self.lists[eng].append(("op", fn, None, 0))
        self._mark(tick, reads, writes)
        return tick

    def load(self, eng, fn, dst, reads=()):
        self._need(eng, self._deps(reads, [dst]))
        sem = self._dsem(dst)
        dst.dcount += 16
        tick = (sem, dst.dcount)
        self.lists[eng].append(("op", fn, sem, 16))
        self.ninst += 1
        self._mark(tick, reads, [dst])
        return tick

    def store(self, eng, fn, src, writes=(), final=False):
        self._need(eng, self._deps([src], writes))
        sem = self._dsem(src)
        src.dcount += 16
        tick = (sem, src.dcount)
        self.lists[eng].append(("op", fn, sem, 16))
        self.ninst += 1
        self._mark(tick, [src], writes)
        self.stores[id(sem)] = tick
        return tick

    def barrier(self):
        deps = [(self.sem[e], self.count[e]) for e in self.ENGS if self.count[e] > 0]
        for b, i in self.stage_bufs:
            deps.append((b.dsem, b.dcount))
        for e in self.ENGS:
            own = self.sem[e]
            self._need(e, [d for d in deps if not (e == "tensor" and d[0] is own)])

    def end_stage(self):
        self.barrier()
        for b, i in self.stage_bufs:
            self.pool[i][1] = b.dcount
            self.free.append(i)
        self.stage_bufs = []
        self.stores = {}
        self.emit()

    def emit(self):
        lists = self.lists
        self.lists = {e: [] for e in self.ENGS}
        with self.nc.Block() as block:
            def run(eng_name):
                def body(e):
                    for it in lists[eng_name]:
                        if it[0] == "wait":
                            e.wait_ge(it[1], it[2])
                        elif it[2] is None:
                            it[1](e)
                        else:
                            it[1](e).then_inc(it[2], it[3])
                return body
            block.sync(run("sync"))
            block.gpsimd(run("gpsimd"))
            block.scalar(run("scalar"))
            block.vector(run("vector"))
            block.tensor(run("tensor"))


_UID = [0]


def _sb(nc, es, name, shape, dt):
    _UID[0] += 1
    return es.enter_context(nc.sbuf_tensor("%s_u%d" % (name, _UID[0]), list(shape), dt))


def _ps(nc, es, name, shape=(128, 512), dt=F32):
    return es.enter_context(nc.psum_tensor(name, list(shape), dt))


FM_COLS = [0, 128, 256, 384, 512, 640, 896] + [2066 + 128 * i for i in range(6)]
RW_COLS = [1170 + 128 * i for i in range(7)]
MNEG = -8000.0


class G:
    pass


def setup_globals(nc, es, S, debug=False):
    g = G()
    g.nc, g.es, g.S = nc, es, S
    g.P = Prog(nc, es)
    P = g.P
    g.NQ = S // 128
    d = lambda name, shape, dt: nc.dram_tensor(name, list(shape), dt, kind=("ExternalOutput" if debug else "Internal")).ap()
    g.FM = d("FM", [13 * 128, S], BF16)
    g.RW = d("RW", [896, S], F32)
    g.TMV = d("TMV", [S, 640], BF16)
    g.GT = d("GT", [S, 18], F32)
    g.OMT = d("OMT", [1024, S], BF16)
    g.X = d("X", [D_MODEL, S], F32)
    g.ps = [_ps(nc, es, "ps%d" % i) for i in range(8)]
    g.bps = [P.buf("ps%d" % i) for i in range(8)]
    g.cb = _sb(nc, es, "cb", [128, 4, 128], BF16)
    g.bcb = P.buf("cb")
    g.cf = _sb(nc, es, "cf", [128, 2, 128], F32)
    g.bcf = P.buf("cf")
    return g


def host_consts():
    import ml_dtypes
    i = np.arange(128)
    cb = np.zeros((128, 4, 128), np.float32)
    cb[:, 0, :] = np.eye(128)
    cb[:, 1, :] = np.where(i[:, None] <= i[None, :], 0.0, MNEG)
    cb[:, 2, :] = np.where(i[:, None] > i[None, :], 0.0, MNEG)
    cb[:, 3, :] = 1.0
    cf = np.zeros((128, 2, 128), np.float32)
    cf[:, 0, :] = np.eye(128)
    cf[:, 1, :] = 1.0
    return {"c_cb": cb.astype(ml_dtypes.bfloat16), "c_cf": cf}


def host_consts_s(S):
    import ml_dtypes
    key = np.arange(S)
    ohm = (key[None, :] // 256 == np.arange(32)[:, None]).astype(np.float32)
    return {"c_ohm": ohm.astype(ml_dtypes.bfloat16)}


def load_consts(g):
    nc, P = g.nc, g.P
    c_cb = nc.dram_tensor("c_cb", [128, 4, 128], BF16, kind="ExternalInput").ap()
    c_cf = nc.dram_tensor("c_cf", [128, 2, 128], F32, kind="ExternalInput").ap()
    P.load("sync", lambda e: e.dma_start(out=g.cb[:], in_=c_cb[:, :, :]), g.bcb)
    P.load("sync", lambda e: e.dma_start(out=g.cf[:], in_=c_cf[:, :, :]), g.bcf)


def stage_p1(g, x_src, gain_ap, w_ap):
    nc, P, S = g.nc, g.P, g.S
    KC = 8
    NT = 512
    with ExitStack() as st:
        W = _sb(nc, st, "p1W", [128, KC, N_IN], BF16)
        gs = _sb(nc, st, "p1gs", [128, KC], F32)
        stg = [_sb(nc, st, "p1stg%d" % i, [128, N_IN], F32) for i in range(2)]
        xs = [_sb(nc, st, "p1xs%d" % i, [128, KC, NT], F32) for i in range(2)]
        hs = [_sb(nc, st, "p1hs%d" % i, [128, KC, NT], BF16) for i in range(2)]
        sq = [_sb(nc, st, "p1sq%d" % i, [128, KC, NT], BF16) for i in range(2)]
        rb = [_sb(nc, st, "p1rb%d" % i, [128, NT], F32) for i in range(2)]
        rc = [_sb(nc, st, "p1rc%d" % i, [128, 4], F32) for i in range(2)]
        ofm = [_sb(nc, st, "p1ofm%d" % i, [128, NT], BF16) for i in range(4)]
        orw = [_sb(nc, st, "p1orw%d" % i, [128, NT], F32) for i in range(4)]
        otm = [_sb(nc, st, "p1otm%d" % i, [128, 640], BF16) for i in range(2)]
        ogt = [_sb(nc, st, "p1ogt%d" % i, [128, 18], F32) for i in range(2)]
        B = P.buf
        bW = [B("W%d" % k) for k in range(KC)]
        bgs = B("gs")
        bstg = [B("stg0"), B("stg1")]
        bxs, bhs, bsq, brb, brc = ([B("b%d" % i) for i in range(2)] for _ in range(5))
        bofm = [B("ofm%d" % i) for i in range(4)]
        borw = [B("orw%d" % i) for i in range(4)]
        botm = [B("otm%d" % i) for i in range(2)]
        bogt = [B("ogt%d" % i) for i in range(2)]
        ps, bps = g.ps, g.bps

        P.load("sync", lambda e: e.dma_start(out=gs[:], in_=gain_ap.rearrange("(k p) -> p k", p=128), allow_slow_non_contiguous=True), bgs)
        for k in range(KC):
            s = k % 2
            P.load("sync" if k % 2 == 0 else "gpsimd",
                   lambda e, k=k, s=s: e.dma_start(out=stg[s][:], in_=w_ap[k * 128:(k + 1) * 128, :]), bstg[s])
            P.op("vector" if k % 2 == 0 else "vector",
                 lambda e, k=k, s=s: e.tensor_scalar(W[:, k, :], stg[s][:], gs[:, k:k + 1], None, ALU.mult),
                 reads=[bstg[s], bgs], writes=[bW[k]])
        import os
        DBG = int(os.environ.get("DBG", "9"))
        xv = x_src.rearrange("(k p) t -> p k t", p=128)
        pi = 0
        oi = 0
        for t in range(S // NT if DBG >= 2 else 0):
            s = t % 2
            t0 = t * NT
            for k in range(KC):
                P.load("sync" if k % 2 == 0 else "gpsimd", lambda e, s=s, t0=t0, k=k: e.dma_start(out=xs[s][:, k, :], in_=x_src[k * 128:(k + 1) * 128, t0:t0 + NT]), bxs[s])
            P.op("scalar", lambda e, s=s: e.activation(out=sq[s][:], in_=xs[s][:], func=AF.Square), reads=[bxs[s]], writes=[bsq[s]])
            P.op("vector", lambda e, s=s: e.tensor_copy(hs[s][:], xs[s][:]), reads=[bxs[s]], writes=[bhs[s]])
            for k in range(KC):
                P.op("tensor", lambda e, k=k, s=s: e.matmul(ps[0][:, 0:NT], g.cb[:, 3, :], sq[s][:, k, :], start=(k == 0), stop=(k == KC - 1)),
                     reads=[bsq[s], g.bcb], writes=[bps[0]], inc=(k == KC - 1))
            for j in range(4):
                for k in range(KC):
                    P.op("tensor", lambda e, k=k, s=s, j=j: e.matmul(ps[1][:, j:j + 1], sq[s][:, k, j * 128:(j + 1) * 128], g.cb[:, 3, 0:1],
                                                                   start=(j == 0 and k == 0), stop=(k == KC - 1), skip_group_check=True),
                         reads=[bsq[s], g.bcb], writes=[bps[1]], inc=(j == 3 and k == KC - 1))
            P.op("scalar", lambda e, s=s: e.activation(out=rb[s][:], in_=ps[0][:, 0:NT], func=AF.Sqrt, scale=1.0 / D_MODEL, bias=NORM_EPS),
                 reads=[bps[0]], writes=[brb[s]])
            P.op("vector", lambda e, s=s: e.reciprocal(rb[s][:], rb[s][:]), reads=[brb[s]], writes=[brb[s]])
            P.op("scalar", lambda e, s=s: e.activation(out=rc[s][:], in_=ps[1][:, 0:4], func=AF.Sqrt, scale=1.0 / D_MODEL, bias=NORM_EPS),
                 reads=[bps[1]], writes=[brc[s]])
            P.op("vector", lambda e, s=s: e.reciprocal(rc[s][:], rc[s][:]), reads=[brc[s]], writes=[brc[s]])
            for ci, c0 in enumerate(FM_COLS + RW_COLS if DBG >= 3 else []):
                pb = 2 + pi % 6
                pi += 1
                for k in range(KC):
                    P.op("tensor", lambda e, k=k, s=s, pb=pb, c0=c0: e.matmul(ps[pb][:, 0:NT], W[:, k, c0:c0 + 128], hs[s][:, k, :], start=(k == 0), stop=(k == KC - 1)),
                         reads=[bhs[s], bW[k]], writes=[bps[pb]], inc=(k == KC - 1))
                o = oi % 4
                oi += 1
                if ci < 13:
                    P.op("vector", lambda e, s=s, pb=pb, o=o: e.tensor_tensor(ofm[o][:], ps[pb][:, 0:NT], rb[s][:], ALU.mult),
                         reads=[bps[pb], brb[s]], writes=[bofm[o]])
                    P.store("sync", lambda e, o=o, ci=ci, t0=t0: e.dma_start(out=g.FM[ci * 128:(ci + 1) * 128, t0:t0 + NT], in_=ofm[o][:]), bofm[o])
                else:
                    ri = ci - 13
                    P.op("vector", lambda e, s=s, pb=pb, o=o: e.tensor_tensor(orw[o][:], ps[pb][:, 0:NT], rb[s][:], ALU.mult),
                         reads=[bps[pb], brb[s]], writes=[borw[o]])
                    P.store("sync", lambda e, o=o, ri=ri, t0=t0: e.dma_start(out=g.RW[ri * 128:(ri + 1) * 128, t0:t0 + NT], in_=orw[o][:]), borw[o])
            for j in range(4 if DBG >= 4 else 0):
                o = j % 2
                pa = 2 + pi % 6
                pi += 1
                pb = 2 + pi % 6
                pi += 1
                for k in range(KC):
                    P.op("tensor", lambda e, k=k, s=s, j=j, pa=pa: e.matmul(ps[pa][:, 0:128], hs[s][:, k, j * 128:(j + 1) * 128], W[:, k, 768:896],
                                                                          start=(k == 0), stop=(k == KC - 1), skip_group_check=True),
                         reads=[bhs[s], bW[k]], writes=[bps[pa]], inc=False)
                for k in range(KC):
                    P.op("tensor", lambda e, k=k, s=s, j=j, pa=pa: e.matmul(ps[pa][:, 128:274], hs[s][:, k, j * 128:(j + 1) * 128], W[:, k, 1024:1170],
                                                                          start=False, stop=(k == KC - 1), skip_group_check=True),
                         reads=[bhs[s], bW[k]], writes=[bps[pa]], inc=(k == KC - 1))
                for k in range(KC):
                    P.op("tensor", lambda e, k=k, s=s, j=j, pb=pb: e.matmul(ps[pb][:, 0:384], hs[s][:, k, j * 128:(j + 1) * 128], W[:, k, 2834:3218],
                                                                          start=(k == 0), stop=(k == KC - 1)),
                         reads=[bhs[s], bW[k]], writes=[bps[pb]], inc=(k == KC - 1))
                P.op("scalar", lambda e, s=s, j=j, pa=pa, o=o: e.activation(out=otm[o][:, 0:256], in_=ps[pa][:, 0:256], func=AF.Copy, scale=rc[s][:, j:j + 1]),
                     reads=[bps[pa], brc[s]], writes=[botm[o]])
                P.op("scalar", lambda e, s=s, j=j, pa=pa, o=o: e.activation(out=ogt[o][:], in_=ps[pa][:, 256:274], func=AF.Copy, scale=rc[s][:, j:j + 1]),
                     reads=[bps[pa], brc[s]], writes=[bogt[o]])
                P.op("scalar", lambda e, s=s, j=j, pb=pb, o=o: e.activation(out=otm[o][:, 256:640], in_=ps[pb][:, 0:384], func=AF.Copy, scale=rc[s][:, j:j + 1]),
                     reads=[bps[pb], brc[s]], writes=[botm[o]])
                r0 = t0 + j * 128
                P.store("gpsimd", lambda e, o=o, r0=r0: e.dma_start(out=g.TMV[r0:r0 + 128, :], in_=otm[o][:]), botm[o])
                P.store("gpsimd", lambda e, o=o, r0=r0: e.dma_start(out=g.GT[r0:r0 + 128, :], in_=ogt[o][:]), bogt[o])
        P.end_stage()


def _load_cast_rows(P, nc, st, name, dst_tile, dst_bufs, src_ap, nrow_chunks, ncols, colblk, scale_ap=None, scale_buf=None):
    stg = [_sb(nc, st, "%s_stg%d" % (name, i), [128, colblk], F32) for i in range(2)]
    bstg = [P.buf("%s_stg%d" % (name, i)) for i in range(2)]
    i = 0
    for r in range(nrow_chunks):
        for c0 in range(0, ncols, colblk):
            c1 = min(ncols, c0 + colblk)
            s = i % 2
            i += 1
            P.load("sync" if s == 0 else "gpsimd",
                   lambda e, r=r, c0=c0, c1=c1, s=s: e.dma_start(out=stg[s][:, 0:c1 - c0], in_=src_ap[r * 128:(r + 1) * 128, c0:c1]), bstg[s])
            if scale_ap is None:
                if s == 0:
                    P.op("vector", lambda e, r=r, c0=c0, c1=c1, s=s: e.tensor_copy(dst_tile[:, r, c0:c1], stg[s][:, 0:c1 - c0]),
                         reads=[bstg[s]], writes=[dst_bufs[r]])
                else:
                    P.op("scalar", lambda e, r=r, c0=c0, c1=c1, s=s: e.activation(out=dst_tile[:, r, c0:c1], in_=stg[s][:, 0:c1 - c0], func=AF.Copy),
                         reads=[bstg[s]], writes=[dst_bufs[r]])
            else:
                P.op("vector", lambda e, r=r, c0=c0, c1=c1, s=s: e.tensor_scalar(dst_tile[:, r, c0:c1], stg[s][:, 0:c1 - c0], scale_ap[:, r:r + 1], None, ALU.mult),
                     reads=[bstg[s], scale_buf], writes=[dst_bufs[r]])


def stage_p3a(g, x_src, wout_ap):
    nc, P, S = g.nc, g.P, g.S
    KC, NT = 8, 512
    ps, bps = g.ps, g.bps
    with ExitStack() as st:
        Wo = _sb(nc, st, "p3Wo", [128, KC, D_MODEL], BF16)
        bWo = [P.buf("Wo%d" % k) for k in range(KC)]
        _load_cast_rows(P, nc, st, "p3a", Wo, bWo, wout_ap, KC, D_MODEL, D_MODEL)
        xt = [_sb(nc, st, "p3xt%d" % i, [128, KC, NT], F32) for i in range(2)]
        om = [_sb(nc, st, "p3om%d" % i, [128, KC, NT], BF16) for i in range(2)]
        bxt = [P.buf("xt%d" % i) for i in range(2)]
        bom = [P.buf("om%d" % i) for i in range(2)]
        xv = x_src.rearrange("(k p) t -> p k t", p=128)
        ov = g.OMT.rearrange("(k p) t -> p k t", p=128)
        xo = g.X.rearrange("(k p) t -> p k t", p=128)
        pi = 0
        for t in range(S // NT):
            s = t % 2
            t0 = t * NT
            for k in range(KC):
                P.load("sync", lambda e, s=s, t0=t0, k=k: e.dma_start(out=xt[s][:, k, :], in_=x_src[k * 128:(k + 1) * 128, t0:t0 + NT]), bxt[s])
                P.load("gpsimd", lambda e, s=s, t0=t0, k=k: e.dma_start(out=om[s][:, k, :], in_=g.OMT[k * 128:(k + 1) * 128, t0:t0 + NT]), bom[s])
            for m in range(KC):
                pb = pi % 8
                pi += 1
                for k in range(KC):
                    P.op("tensor", lambda e, k=k, m=m, s=s, pb=pb: e.matmul(ps[pb][:, 0:NT], Wo[:, k, m * 128:(m + 1) * 128], om[s][:, k, :], start=(k == 0), stop=(k == KC - 1)),
                         reads=[bom[s], bWo[k]], writes=[bps[pb]], inc=(k == KC - 1))
                P.op("vector", lambda e, m=m, s=s, pb=pb: e.tensor_tensor(xt[s][:, m, :], ps[pb][:, 0:NT], xt[s][:, m, :], ALU.add),
                     reads=[bps[pb], bxt[s]], writes=[bxt[s]])
            for k in range(KC):
                P.store("sync" if k % 2 == 0 else "gpsimd", lambda e, s=s, t0=t0, k=k: e.dma_start(out=g.X[k * 128:(k + 1) * 128, t0:t0 + NT], in_=xt[s][:, k, :]), bxt[s])
        P.end_stage()


def stage_p3b(g, gain_ap, win_ap, cw_ap, cbias_ap, wff_ap):
    nc, P, S = g.nc, g.P, g.S
    KC, FC, NT = 8, D_FF // 128, 256
    ps, bps = g.ps, g.bps
    with ExitStack() as st:
        Wi = _sb(nc, st, "p3Wi", [128, KC, 2 * D_FF], BF16)
        Wf = _sb(nc, st, "p3Wf", [128, FC, D_MODEL], BF16)
        gs = _sb(nc, st, "p3gs", [128, KC], F32)
        cw = _sb(nc, st, "p3cw", [128, 3, FC], F32)
        cbs = _sb(nc, st, "p3cb", [128, FC], F32)
        bWi = [P.buf("Wi%d" % k) for k in range(KC)]
        bWf = [P.buf("Wf%d" % k) for k in range(FC)]
        bsm = P.buf("small")
        P.load("sync", lambda e: e.dma_start(out=gs[:], in_=gain_ap.rearrange("(k p) -> p k", p=128), allow_slow_non_contiguous=True), bsm)
        P.load("sync", lambda e: e.dma_start(out=cw[:], in_=cw_ap.rearrange("j (c p) -> p j c", p=128), allow_slow_non_contiguous=True), bsm)
        P.load("sync", lambda e: e.dma_start(out=cbs[:], in_=cbias_ap.rearrange("(c p) -> p c", p=128), allow_slow_non_contiguous=True), bsm)
        with ExitStack() as st2:
            _load_cast_rows(P, nc, st2, "p3bi", Wi, bWi, win_ap, KC, 2 * D_FF, 1408, scale_ap=gs, scale_buf=bsm)
            _load_cast_rows(P, nc, st2, "p3bf", Wf, bWf, wff_ap, FC, D_MODEL, D_MODEL)
            P.barrier()
            P.emit()
        xt = [_sb(nc, st, "p3bxt%d" % i, [128, KC, NT], F32) for i in range(2)]
        sq = _sb(nc, st, "p3bsq", [128, KC, NT], BF16)
        h2 = _sb(nc, st, "p3bh2", [128, KC, NT], BF16)
        rb = _sb(nc, st, "p3brb", [128, NT], F32)
        act = _sb(nc, st, "p3bact", [128, FC, NT], BF16)
        gb = [_sb(nc, st, "p3bgb%d" % i, [128, NT + 2], F32) for i in range(2)]
        acc = [_sb(nc, st, "p3bacc%d" % i, [128, NT], F32) for i in range(2)]
        carry = _sb(nc, st, "p3bcar", [128, FC, 2], F32)
        bxt = [P.buf("xt%d" % i) for i in range(2)]
        bsq, bh2, brb = P.buf("sq"), P.buf("h2"), P.buf("rb")
        bact = [P.buf("act%d" % c) for c in range(FC)]
        bgb = [P.buf("gb%d" % i) for i in range(2)]
        bacc = [P.buf("acc%d" % i) for i in range(2)]
        bcar = [P.buf("car%d" % c) for c in range(FC)]
        for c in range(FC):
            P.op("vector", lambda e, c=c: e.memset(carry[:, c, :], 0.0), writes=[bcar[c]])
        xv = g.X.rearrange("(k p) t -> p k t", p=128)
        pi = 0
        gi = 0
        for t in range(S // NT):
            s = t % 2
            t0 = t * NT
            for k in range(KC):
                P.load("sync", lambda e, s=s, t0=t0, k=k: e.dma_start(out=xt[s][:, k, :], in_=g.X[k * 128:(k + 1) * 128, t0:t0 + NT]), bxt[s])
            P.op("scalar", lambda e, s=s: e.activation(out=sq[:], in_=xt[s][:], func=AF.Square), reads=[bxt[s]], writes=[bsq])
            for k in range(KC):
                P.op("tensor", lambda e, k=k: e.matmul(ps[0][:, 0:NT], g.cb[:, 3, :], sq[:, k, :], start=(k == 0), stop=(k == KC - 1)),
                     reads=[bsq, g.bcb], writes=[bps[0]], inc=(k == KC - 1))
            P.op("scalar", lambda e: e.activation(out=rb[:], in_=ps[0][:, 0:NT], func=AF.Sqrt, scale=1.0 / D_MODEL, bias=NORM_EPS),
                 reads=[bps[0]], writes=[brb])
            P.op("vector", lambda e: e.reciprocal(rb[:], rb[:]), reads=[brb], writes=[brb])
            for k in range(KC):
                P.op("vector", lambda e, k=k, s=s: e.tensor_tensor(h2[:, k, :], xt[s][:, k, :], rb[:], ALU.mult),
                     reads=[bxt[s], brb], writes=[bh2])
            for c in range(FC):
                pu = 1 + pi % 7
                pi += 1
                pg = 1 + pi % 7
                pi += 1
                q = gi % 2
                gi += 1
                for k in range(KC):
                    P.op("tensor", lambda e, k=k, c=c, pu=pu: e.matmul(ps[pu][:, 0:NT], Wi[:, k, c * 128:(c + 1) * 128], h2[:, k, :], start=(k == 0), stop=(k == KC - 1)),
                         reads=[bh2, bWi[k]], writes=[bps[pu]], inc=(k == KC - 1))
                for k in range(KC):
                    P.op("tensor", lambda e, k=k, c=c, pg=pg: e.matmul(ps[pg][:, 0:NT], Wi[:, k, D_FF + c * 128:D_FF + (c + 1) * 128], h2[:, k, :], start=(k == 0), stop=(k == KC - 1)),
                         reads=[bh2, bWi[k]], writes=[bps[pg]], inc=(k == KC - 1))
                P.op("scalar", lambda e, q=q, pg=pg: e.activation(out=gb[q][:, 2:NT + 2], in_=ps[pg][:, 0:NT], func=AF.Copy), reads=[bps[pg]], writes=[bgb[q]])
                P.op("vector", lambda e, q=q, c=c: e.tensor_copy(gb[q][:, 0:2], carry[:, c, :]), reads=[bcar[c]], writes=[bgb[q]])
                P.op("vector", lambda e, q=q, c=c: e.tensor_scalar(acc[q][:], gb[q][:, 2:NT + 2], cw[:, 2, c:c + 1], cbs[:, c:c + 1], ALU.mult, ALU.add),
                     reads=[bgb[q], bsm], writes=[bacc[q]])
                P.op("vector", lambda e, q=q, c=c: e.scalar_tensor_tensor(acc[q][:], gb[q][:, 1:NT + 1], cw[:, 1, c:c + 1], acc[q][:], ALU.mult, ALU.add),
                     reads=[bgb[q], bsm, bacc[q]], writes=[bacc[q]])
                P.op("vector", lambda e, q=q, c=c: e.scalar_tensor_tensor(acc[q][:], gb[q][:, 0:NT], cw[:, 0, c:c + 1], acc[q][:], ALU.mult, ALU.add),
                     reads=[bgb[q], bsm, bacc[q]], writes=[bacc[q]])
                P.op("vector", lambda e, q=q, c=c: e.tensor_copy(carry[:, c, :], gb[q][:, NT:NT + 2]), reads=[bgb[q]], writes=[bcar[c]])
                P.op("scalar", lambda e, q=q: e.activation(out=acc[q][:], in_=acc[q][:], func=AF.Silu), reads=[bacc[q]], writes=[bacc[q]])
                P.op("vector", lambda e, q=q, c=c, pu=pu: e.tensor_tensor(act[:, c, :], ps[pu][:, 0:NT], acc[q][:], ALU.mult),
                     reads=[bps[pu], bacc[q]], writes=[bact[c]])
            for m in range(KC):
                pb = 1 + pi % 7
                pi += 1
                for c in range(FC):
                    P.op("tensor", lambda e, c=c, m=m, pb=pb: e.matmul(ps[pb][:, 0:NT], Wf[:, c, m * 128:(m + 1) * 128], act[:, c, :], start=(c == 0), stop=(c == FC - 1)),
                         reads=[bact[c], bWf[c]], writes=[bps[pb]], inc=(c == FC - 1))
                P.op("vector", lambda e, m=m, s=s, pb=pb: e.tensor_tensor(xt[s][:, m, :], ps[pb][:, 0:NT], xt[s][:, m, :], ALU.add),
                     reads=[bps[pb], bxt[s]], writes=[bxt[s]])
            for k in range(KC):
                P.store("gpsimd" if k % 2 == 0 else "sync", lambda e, s=s, t0=t0, k=k: e.dma_start(out=g.X[k * 128:(k + 1) * 128, t0:t0 + NT], in_=xt[s][:, k, :]), bxt[s])
        P.end_stage()


def stage_final(g, gain_ap, out_ap):
    nc, P, S = g.nc, g.P, g.S
    KC, NT = 8, 512
    ps, bps = g.ps, g.bps
    with ExitStack() as st:
        gs = _sb(nc, st, "fngs", [128, KC], F32)
        bgs = P.buf("gs")
        P.load("sync", lambda e: e.dma_start(out=gs[:], in_=gain_ap.rearrange("(k p) -> p k", p=128), allow_slow_non_contiguous=True), bgs)
        xt = [_sb(nc, st, "fnxt%d" % i, [128, KC, NT], F32) for i in range(2)]
        sq = [_sb(nc, st, "fnsq%d" % i, [128, KC, NT], BF16) for i in range(2)]
        rb = [_sb(nc, st, "fnrb%d" % i, [128, NT], F32) for i in range(2)]
        bxt, bsq, brb = ([P.buf("f%d" % i) for i in range(2)] for _ in range(3))
        xv = g.X.rearrange("(k p) t -> p k t", p=128)
        ov = out_ap.rearrange("(k p) t -> p k t", p=128)
        for t in range(S // NT):
            s = t % 2
            t0 = t * NT
            pb = t % 2
            for k in range(KC):
                P.load("sync", lambda e, s=s, t0=t0, k=k: e.dma_start(out=xt[s][:, k, :], in_=g.X[k * 128:(k + 1) * 128, t0:t0 + NT]), bxt[s])
            P.op("scalar", lambda e, s=s: e.activation(out=sq[s][:], in_=xt[s][:], func=AF.Square), reads=[bxt[s]], writes=[bsq[s]])
            for k in range(KC):
                P.op("tensor", lambda e, k=k, s=s, pb=pb: e.matmul(ps[pb][:, 0:NT], g.cb[:, 3, :], sq[s][:, k, :], start=(k == 0), stop=(k == KC - 1)),
                     reads=[bsq[s], g.bcb], writes=[bps[pb]], inc=(k == KC - 1))
            P.op("scalar", lambda e, s=s, pb=pb: e.activation(out=rb[s][:], in_=ps[pb][:, 0:NT], func=AF.Sqrt, scale=1.0 / D_MODEL, bias=NORM_EPS),
                 reads=[bps[pb]], writes=[brb[s]])
            P.op("vector", lambda e, s=s: e.reciprocal(rb[s][:], rb[s][:]), reads=[brb[s]], writes=[brb[s]])
            for k in range(KC):
                P.op("vector", lambda e, k=k, s=s: e.scalar_tensor_tensor(xt[s][:, k, :], xt[s][:, k, :], gs[:, k:k + 1], rb[s][:], ALU.mult, ALU.mult),
                     reads=[bxt[s], brb[s], bgs], writes=[bxt[s]])
            for k in range(KC):
                P.store("gpsimd" if k % 2 == 0 else "sync", lambda e, s=s, t0=t0, k=k: e.dma_start(out=out_ap[k * 128:(k + 1) * 128, t0:t0 + NT], in_=xt[s][:, k, :]), bxt[s])
        P.end_stage()


def stage_moba(g, gain_ap, c_ohm):
    nc, P, S, NQ = g.nc, g.P, g.S, g.NQ
    NB = S // 256
    ps, bps = g.ps, g.bps
    ptb = g.ps[7][:].bitcast(BF16)
    with ExitStack() as st:
        KX = _sb(nc, st, "mbKX", [128, S], BF16)
        QX = _sb(nc, st, "mbQX", [128, S], BF16)
        Vp = _sb(nc, st, "mbVp", [128, NQ, 65], BF16)
        kmf = _sb(nc, st, "mbkmf", [64, NB], F32)
        kmb = _sb(nc, st, "mbkmb", [64, NB], BF16)
        G32 = _sb(nc, st, "mbG32", [128, 32], F32)
        m8 = _sb(nc, st, "mbm8", [128, 8], F32)
        NMW = _sb(nc, st, "mbNMW", [128, 96], BF16)
        PT = [_sb(nc, st, "mbPT%d" % i, [128, 512], BF16) for i in range(3)]
        gainb = _sb(nc, st, "mbgain", [128, 384], F32)
        fs = [_sb(nc, st, "mbfs%d" % i, [128, 4], F32) for i in range(2)]
        o32 = [_sb(nc, st, "mbo32%d" % i, [128, 64], F32) for i in range(2)]
        junk = _sb(nc, st, "mbjunk", [128, 64], F32)
        ob = [_sb(nc, st, "mbob%d" % i, [128, 64], BF16) for i in range(2)]
        OT = [_sb(nc, st, "mbOT%d" % i, [64, 512], BF16) for i in range(2)]
        B = P.buf
        bKXk, bKXo, bQXq, bQXm, bVp, bVp1 = B("KXk"), B("KXo"), B("QXq"), B("QXm"), B("Vp"), B("Vp1")
        bkm, bG32, bm8, bNMW, bgain = B("km"), B("G32"), B("m8"), B("NMW"), B("gain")
        bPT = [B("PT%d" % i) for i in range(3)]
        bfs = [B("fs%d" % i) for i in range(2)]
        bo32 = [B("o32%d" % i) for i in range(2)]
        bjunk = B("junk")
        bob = [B("ob%d" % i) for i in range(2)]
        bOT = [B("OT%d" % i) for i in range(2)]

        P.load("sync", lambda e: e.dma_start(out=KX[64:96, :], in_=c_ohm[:, :]), bKXo)
        P.load("sync", lambda e: e.dma_start(out=gainb[:], in_=gain_ap.partition_broadcast(128), allow_slow_non_contiguous=True), bgain)
        P.op("vector", lambda e: e.memset(QX[64:96, :], 0.0), writes=[bQXm])
        P.op("vector", lambda e: e.memset(Vp[:, :, 64:65], 1.0), writes=[bVp1])
        tv = g.TMV.rearrange("(t p) d -> p t d", p=128)
        pti = 0
        sbank = 0
        for h in range(6):
            P.load("sync", lambda e, h=h: e.dma_start(out=KX[0:64, :], in_=g.FM[10 * 128 + h * 64:10 * 128 + (h + 1) * 64, :]), bKXk)
            P.load("gpsimd", lambda e, h=h: e.dma_start(out=QX[0:64, :], in_=g.FM[7 * 128 + h * 64:7 * 128 + (h + 1) * 64, :]), bQXq)
            for t_ in range(0, NQ, 16):
                P.load("sync", lambda e, h=h, t_=t_: e.dma_start(out=Vp[:, t_:t_ + 16, 0:64], in_=tv[:, t_:t_ + 16, 256 + h * 64:256 + (h + 1) * 64]), bVp)
            P.op("vector", lambda e: e.tensor_reduce(kmf[:, :], KX[0:64, :].rearrange("p (n k) -> p n k", k=256), AX.X, ALU.add),
                 reads=[bKXk], writes=[bkm])
            P.op("vector", lambda e: e.tensor_scalar(kmb[:, :], kmf[:, :], 1.0 / 256, None, ALU.mult), reads=[bkm], writes=[bkm])
            P.op("vector", lambda e: e.memset(G32[:], -1e30), writes=[bG32])
            P.op("vector", lambda e: e.memset(NMW[:], 0.0), writes=[bNMW])
            for qt in range(NQ):
                cur = qt // 2
                if cur == 0:
                    continue
                qs = slice(qt * 128, (qt + 1) * 128)
                P.op("tensor", lambda e, qs=qs: e.matmul(ps[6][:, 0:NB], QX[0:64, qs], kmb[0:64, 0:NB], start=True, stop=True),
                     reads=[bQXq, bkm], writes=[bps[6]])
                P.op("vector", lambda e, cur=cur: e.tensor_copy(G32[:, 0:cur], ps[6][:, 0:cur]), reads=[bps[6]], writes=[bG32])
                P.op("vector", lambda e: e.max(out=m8[:], in_=G32[:, 0:32]), reads=[bG32], writes=[bm8])
                P.op("vector", lambda e, cur=cur: e.tensor_scalar(NMW[:, 64:64 + cur], G32[:, 0:cur], m8[:, 2:3], MNEG, ALU.is_lt, ALU.mult),
                     reads=[bG32, bm8], writes=[bNMW])
                P.op("tensor", lambda e: e.transpose(ptb[0:96, 0:128], NMW[:, 0:96], g.cb[:, 0, :]), reads=[bNMW, g.bcb], writes=[bps[7]])
                P.op("scalar", lambda e, qs=qs: e.activation(out=QX[64:96, qs], in_=ptb[64:96, 0:128], func=AF.Copy), reads=[bps[7]], writes=[bQXm])
            for qt in range(NQ):
                qs = slice(qt * 128, (qt + 1) * 128)
                ob_ = 4 + qt % 2
                f = qt % 2
                for g0 in range(0, qt + 1, 4):
                    kts = list(range(g0, min(g0 + 4, qt + 1)))
                    bank = sbank % 4
                    sbank += 1
                    mms = []
                    for j, kt in enumerate(kts):
                        diag = (kt == qt)
                        mms.append((lambda e, j=j, kt=kt, qs=qs, bank=bank, diag=diag: e.matmul(
                            ps[bank][:, j * 128:(j + 1) * 128], KX[0:96, kt * 128:(kt + 1) * 128], QX[0:96, qs],
                            start=(j == 0), stop=(not diag), skip_group_check=True), [bKXk, bKXo, bQXq, bQXm]))
                        if diag:
                            mms.append((lambda e, j=j, bank=bank: e.matmul(
                                ps[bank][:, j * 128:(j + 1) * 128], g.cb[:, 0, :], g.cb[:, 1, :], start=False, stop=True, skip_group_check=True),
                                [g.bcb]))
                    for i_, (fn_, rd_) in enumerate(mms):
                        P.op("tensor", fn_, reads=rd_, writes=[bps[bank]], inc=(i_ == len(mms) - 1))
                    n = len(kts)
                    pt = pti % 3
                    pti += 1
                    P.op("scalar", lambda e, bank=bank, n=n, pt=pt: e.activation(out=PT[pt][:, 0:n * 128], in_=ps[bank][:, 0:n * 128], func=AF.Exp, scale=0.125),
                         reads=[bps[bank]], writes=[bPT[pt]])
                    for j, kt in enumerate(kts):
                        P.op("tensor", lambda e, j=j, kt=kt, pt=pt, ob_=ob_, qt=qt: e.matmul(
                            ps[ob_][:, 0:65], PT[pt][:, j * 128:(j + 1) * 128], Vp[:, kt, :], start=(kt == 0), stop=(kt == qt)),
                            reads=[bPT[pt], bVp, bVp1], writes=[bps[ob_]], inc=(kt == qt or j == len(kts) - 1))
                P.op("vector", lambda e, f=f, ob_=ob_: e.tensor_scalar(fs[f][:, 0:1], ps[ob_][:, 64:65], 1e-30, None, ALU.max), reads=[bps[ob_]], writes=[bfs[f]])
                P.op("vector", lambda e, f=f: e.reciprocal(fs[f][:, 0:1], fs[f][:, 0:1]), reads=[bfs[f]], writes=[bfs[f]])
                P.op("vector", lambda e, f=f, ob_=ob_: e.tensor_scalar(o32[f][:], ps[ob_][:, 0:64], fs[f][:, 0:1], None, ALU.mult),
                     reads=[bps[ob_], bfs[f]], writes=[bo32[f]])
                P.op("scalar", lambda e, f=f: e.activation(out=junk[:], in_=o32[f][:], func=AF.Square, accum_out=fs[f][:, 1:2]),
                     reads=[bo32[f]], writes=[bjunk, bfs[f]])
                P.op("scalar", lambda e, f=f: e.activation(out=fs[f][:, 2:3], in_=fs[f][:, 1:2], func=AF.Sqrt, scale=1.0 / 64, bias=NORM_EPS),
                     reads=[bfs[f]], writes=[bfs[f]])
                P.op("vector", lambda e, f=f: e.reciprocal(fs[f][:, 3:4], fs[f][:, 2:3]), reads=[bfs[f]], writes=[bfs[f]])
                P.op("vector", lambda e, f=f, h=h: e.scalar_tensor_tensor(ob[f][:], o32[f][:], fs[f][:, 3:4], gainb[:, h * 64:(h + 1) * 64], ALU.mult, ALU.mult),
                     reads=[bo32[f], bfs[f], bgain], writes=[bob[f]])
                q4 = qt % 4
                P.op("tensor", lambda e, f=f, q4=q4: e.transpose(ptb[0:64, 256 + q4 * 128:256 + (q4 + 1) * 128], ob[f][:], g.cb[:, 0, :]),
                     reads=[bob[f], g.bcb], writes=[bps[7]])
                if q4 == 3:
                    oo = (qt // 4) % 2
                    P.op("scalar", lambda e, oo=oo: e.activation(out=OT[oo][:], in_=ptb[0:64, 256:768], func=AF.Copy), reads=[bps[7]], writes=[bOT[oo]])
                    c0 = (qt - 3) * 128
                    P.store("gpsimd", lambda e, oo=oo, h=h, c0=c0: e.dma_start(out=g.OMT[640 + h * 64:640 + (h + 1) * 64, c0:c0 + 512], in_=OT[oo][:]), bOT[oo])
        P.end_stage()


def host_consts_nsa(S):
    import ml_dtypes
    key = np.arange(S)
    ohs = ((key[None, :] // 64) % 64 == np.arange(64)[:, None]).astype(np.float32)
    n = np.arange(128)
    c = np.arange(2176)
    cm = np.where(16 * n[:, None] + 31 <= c[None, :], 0.0, MNEG).astype(np.float32)
    n_cmp = S // 16 - 1
    n_slc = S // 64
    NCT = (n_cmp + 1 + 127) // 128
    r_, c_ = 4, 2
    ii = (r_ * np.arange(n_slc)[:, None, None] - np.arange(r_)[None, :, None] - np.arange(c_)[None, None, :]).reshape(n_slc, -1)
    mm = (ii[:, :, None] == np.arange(n_cmp)[None, None, :]).sum(1).T.astype(np.float32)
    cmap = np.zeros((NCT * 128, 128), np.float32)
    cmap[:n_cmp, :n_slc] = mm
    cmap = cmap.reshape(NCT, 128, 128).transpose(1, 0, 2)
    BIG = 1e9
    fbw = np.zeros((128, 256), np.float32)
    cc = np.arange(256)
    for i in range(128):
        cur = 128 if i < 64 else 129
        fbw[i] = np.where(cc > cur, -BIG, np.where(cc >= cur - 1, BIG, 0.0))
    return {"c_ohs": ohs.astype(ml_dtypes.bfloat16), "c_cm": cm.astype(ml_dtypes.bfloat16),
            "c_cmap": np.ascontiguousarray(cmap).astype(ml_dtypes.bfloat16), "c_fbw": fbw}


def stage_nsa(g, gain_ap, pos_ap, w1_ap, w2_ap, c_ohs, c_cm, c_cmap, c_fbw):
    nc, P, S, NQ = g.nc, g.P, g.S, g.NQ
    NC = S // 16 - 1
    NCT = (NC + 1 + 127) // 128
    ps, bps = g.ps, g.bps
    ptb = g.ps[7][:].bitcast(BF16)
    B = P.buf
    with ExitStack() as st:
        KSX = _sb(nc, st, "nsKSX", [128, S], BF16)
        KW = _sb(nc, st, "nsKW", [64, S], BF16)
        KVT = _sb(nc, st, "nsKVT", [64, S], BF16)
        VSp = _sb(nc, st, "nsVSp", [128, NQ, 65], BF16)
        VWp = _sb(nc, st, "nsVWp", [128, NQ, 65], BF16)
        KC = _sb(nc, st, "nsKC", [64, NCT * 128], BF16)
        Vcp = _sb(nc, st, "nsVcp", [128, NCT, 65], BF16)
        CM = _sb(nc, st, "nsCM", [128, 2176], BF16)
        CMAP = _sb(nc, st, "nsCMAP", [128, NCT, 128], BF16)
        FBW = _sb(nc, st, "nsFBW", [128, 256], F32)
        TRI = _sb(nc, st, "nsTRI", [128, 2, 384], BF16)
        gainb = _sb(nc, st, "nsgain", [128, 384], F32)
        W1s = _sb(nc, st, "nsW1s", [64, 32, 256], F32)
        W1b = _sb(nc, st, "nsW1b", [64, 32, 256], BF16)
        W2s = _sb(nc, st, "nsW2s", [128, 2, 64], F32)
        W2b = _sb(nc, st, "nsW2b", [128, 2, 64], BF16)
        pss = _sb(nc, st, "nspss", [64, 32], F32)
        psb = _sb(nc, st, "nspsb", [64, 32], BF16)
        bsb = _sb(nc, st, "nsbsb", [128, 2], F32)
        xb = _sb(nc, st, "nsxb", [128, 512], F32)
        x2 = _sb(nc, st, "nsx2", [128, 512], F32)
        ha = _sb(nc, st, "nsha", [128, 2, 512], BF16)
        QXA = [_sb(nc, st, "nsQXA%d" % i, [128, 384], BF16) for i in range(2)]
        QXB = [_sb(nc, st, "nsQXB%d" % i, [128, 384], BF16) for i in range(2)]
        PT = [_sb(nc, st, "nsPT%d" % i, [128, 384], BF16) for i in range(3)]
        gts = [_sb(nc, st, "nsgt%d" % i, [128, 18], F32) for i in range(2)]
        L9 = [_sb(nc, st, "nsL9%d" % i, [128, 9], F32) for i in range(2)]
        C9 = [_sb(nc, st, "nsC9%d" % i, [128, 9], F32) for i in range(2)]
        rlc = _sb(nc, st, "nsrlc", [128, 3], F32)
        imp = _sb(nc, st, "nsimp", [128, 128], F32)
        imp3 = _sb(nc, st, "nsimp3", [128, 128], F32)
        m8a = _sb(nc, st, "nsm8a", [128, 8], F32)
        m8b = _sb(nc, st, "nsm8b", [128, 8], F32)
        W192 = _sb(nc, st, "nsW192", [128, 192], BF16)
        WB = _sb(nc, st, "nsWB", [128, 128], BF16)
        bWB = B("WB")
        fs = [_sb(nc, st, "nsfs%d" % i, [128, 4], F32) for i in range(2)]
        o32 = [_sb(nc, st, "nso32%d" % i, [128, 64], F32) for i in range(2)]
        junk = _sb(nc, st, "nsjunk", [128, 64], F32)
        ob = [_sb(nc, st, "nsob%d" % i, [128, 64], BF16) for i in range(2)]
        OT = [_sb(nc, st, "nsOT%d" % i, [64, 3, 128], BF16) for i in range(2)]
        bKSk, bKSo, bKW, bKVT, bVS, bVW, bV1 = B("KSk"), B("KSo"), B("KW"), B("KVT"), B("VS"), B("VW"), B("V1")
        bKC, bVcp, bconst, bW1s, bW1b, bW2, bpos, bbsb, bxb, bx2, bha = (B("x") for _ in range(11))
        bQXAq = [B("QXAq%d" % i) for i in range(2)]
        bQXAm = [B("QXAm%d" % i) for i in range(2)]
        bQXBq = [B("QXBq%d" % i) for i in range(2)]
        bQXBm = [B("QXBm%d" % i) for i in range(2)]
        bPT = [B("PT%d" % i) for i in range(3)]
        bgts = [B("gt%d" % i) for i in range(2)]
        bL9 = [B("L9%d" % i) for i in range(2)]
        bC9 = [B("C9%d" % i) for i in range(2)]
        brlc, bimp, bimp3, bm8a, bm8b, bW192, bjunk = (B("y") for _ in range(7))
        bfs = [B("fs%d" % i) for i in range(2)]
        bo32 = [B("o32%d" % i) for i in range(2)]
        bob = [B("ob%d" % i) for i in range(2)]
        bOT = [B("OT%d" % i) for i in range(2)]

        P.load("sync", lambda e: e.dma_start(out=KSX[64:128, :], in_=c_ohs[:, :]), bKSo)
        P.load("sync", lambda e: e.dma_start(out=CM[:], in_=c_cm[:, :]), bconst)
        P.load("sync", lambda e: e.dma_start(out=CMAP[:], in_=c_cmap[:, :, :]), bconst)
        P.load("sync", lambda e: e.dma_start(out=FBW[:], in_=c_fbw[:, :]), bconst)
        P.load("sync", lambda e: e.dma_start(out=gainb[:], in_=gain_ap.partition_broadcast(128), allow_slow_non_contiguous=True), bconst)
        for r in range(3):
            P.op("vector", lambda e, r=r: e.tensor_copy(TRI[:, 0, r * 128:(r + 1) * 128], g.cb[:, 1, :]), reads=[g.bcb], writes=[bconst])
            P.op("vector", lambda e, r=r: e.tensor_copy(TRI[:, 1, r * 128:(r + 1) * 128], g.cb[:, 2, :]), reads=[g.bcb], writes=[bconst])
        P.op("vector", lambda e: e.memset(VSp[:, :, 64:65], 1.0), writes=[bV1])
        P.op("vector", lambda e: e.memset(VWp[:, :, 64:65], 1.0), writes=[bV1])
        P.op("vector", lambda e: e.memset(Vcp[:, :, 64:65], 1.0), writes=[bV1])
        P.op("vector", lambda e: e.memset(ha[:], 0.0), writes=[bha])
        P.op("vector", lambda e: e.memset(W192[:], 0.0), writes=[bW192])
        P.op("vector", lambda e: e.memset(WB[:], 0.0), writes=[bWB])
        tv = g.TMV.rearrange("(t p) d -> p t d", p=128)
        sbank = 0
        pti = 0

        def attn_tile(lhs_fn, lhs_reads, K, rhs_t, rhs_reads, Vt, vreads, kt, obank, ocol, first_o, last_o, masks):
            nonlocal sbank, pti
            bank = sbank % 2
            sbank += 1
            mms = [(lambda e, bank=bank: e.matmul(ps[bank][:, 0:384], lhs_fn(), rhs_t[0:K, 0:384], start=True, stop=(len(masks) == 0), skip_group_check=True),
                    list(lhs_reads) + list(rhs_reads))]
            for (mfn, mreads) in masks:
                mms.append((lambda e, bank=bank, mfn=mfn: mfn(e, ps[bank]), mreads))
            for i_, (fn_, rd_) in enumerate(mms):
                P.op("tensor", fn_, reads=rd_, writes=[bps[bank]], inc=(i_ == len(mms) - 1))
            pt = pti % 3
            pti += 1
            P.op("scalar", lambda e, bank=bank, pt=pt: e.activation(out=PT[pt][:], in_=ps[bank][:, 0:384], func=AF.Exp, scale=0.125),
                 reads=[bps[bank]], writes=[bPT[pt]])
            for r in range(3):
                P.op("tensor", lambda e, r=r, pt=pt: e.matmul(ps[obank][:, ocol + r * 65:ocol + (r + 1) * 65], PT[pt][:, r * 128:(r + 1) * 128], Vt,
                                                             start=(first_o and r == 0), stop=last_o, skip_group_check=True),
                     reads=[bPT[pt]] + list(vreads), writes=[bps[obank]], inc=(r == 2))
            return pt

        for g_ in range(2):
            P.load("sync", lambda e, g_=g_: e.dma_start(out=KSX[0:64, :], in_=g.FM[5 * 128 + g_ * 64:5 * 128 + (g_ + 1) * 64, :]), bKSk)
            P.load("gpsimd", lambda e, g_=g_: e.dma_start(out=KW[:, :], in_=g.FM[6 * 128 + g_ * 64:6 * 128 + (g_ + 1) * 64, :]), bKW)
            for t_ in range(0, NQ, 16):
                P.load("sync", lambda e, g_=g_, t_=t_: e.dma_start(out=VSp[:, t_:t_ + 16, 0:64], in_=tv[:, t_:t_ + 16, g_ * 64:(g_ + 1) * 64]), bVS)
                P.load("gpsimd", lambda e, g_=g_, t_=t_: e.dma_start(out=VWp[:, t_:t_ + 16, 0:64], in_=tv[:, t_:t_ + 16, 128 + g_ * 64:128 + (g_ + 1) * 64]), bVW)
            for j in range(2):
                P.load("sync", lambda e, g_=g_, j=j: e.dma_start(out=KVT[:, :], in_=g.FM[(3 + j) * 128 + g_ * 64:(3 + j) * 128 + (g_ + 1) * 64, :]), bKVT)
                P.load("gpsimd", lambda e, j=j: e.dma_start(out=W1s[:], in_=w1_ap[j].rearrange("(l d) h -> d l h", d=64)), bW1s)
                P.load("sync", lambda e, j=j: e.dma_start(out=W2s[:], in_=w2_ap[j].rearrange("(c p) d -> p c d", p=128)), bW2)
                P.load("sync", lambda e, j=j: e.dma_start(out=pss[:], in_=pos_ap[j].rearrange("l d -> d l"), allow_slow_non_contiguous=True), bpos)
                P.op("vector", lambda e: e.tensor_copy(W1b[:], W1s[:]), reads=[bW1s], writes=[bW1b])
                P.op("vector", lambda e: e.tensor_copy(W2b[:], W2s[:]), reads=[bW2], writes=[bW2])
                P.op("vector", lambda e: e.tensor_copy(psb[:], pss[:]), reads=[bpos], writes=[bpos])
                for half in range(2):
                    for l in range(32):
                        P.op("tensor", lambda e, half=half, l=l: e.matmul(ps[2][:, half:half + 1], W1b[:, l, half * 128:(half + 1) * 128], psb[:, l:l + 1],
                                                                         start=(half == 0 and l == 0), stop=(l == 31), skip_group_check=True),
                             reads=[bW1b, bpos], writes=[bps[2]], inc=(half == 1 and l == 31))
                P.op("vector", lambda e: e.tensor_copy(bsb[:], ps[2][:, 0:2]), reads=[bps[2]], writes=[bbsb])
                for half in range(2):
                    hb = 5 + half
                    for l in range(32):
                        P.op("tensor", lambda e, half=half, l=l, hb=hb: e.matmul(ps[hb][:, 0:NC], W1b[:, l, half * 128:(half + 1) * 128], KVT[0:64, l:l + 16 * (NC - 1) + 1:16],
                                                                                start=(l == 0), stop=(l == 31)),
                             reads=[bW1b, bKVT], writes=[bps[hb]], inc=(l == 31))
                    P.op("scalar", lambda e, half=half, hb=hb: e.activation(out=xb[:, 0:NC], in_=ps[hb][:, 0:NC], func=AF.Identity, bias=bsb[:, half:half + 1]),
                         reads=[bps[hb], bbsb], writes=[bxb])
                    P.op("scalar", lambda e: e.activation(out=x2[:, 0:NC], in_=xb[:, 0:NC], func=AF.Square), reads=[bxb], writes=[bx2])
                    P.op("vector", lambda e: e.tensor_scalar(x2[:, 0:NC], x2[:, 0:NC], 0.044715, 1.0, ALU.mult, ALU.add), reads=[bx2], writes=[bx2])
                    P.op("vector", lambda e: e.tensor_tensor(x2[:, 0:NC], x2[:, 0:NC], xb[:, 0:NC], ALU.mult), reads=[bx2, bxb], writes=[bx2])
                    P.op("scalar", lambda e: e.activation(out=x2[:, 0:NC], in_=x2[:, 0:NC], func=AF.Sigmoid, scale=1.5957691216), reads=[bx2], writes=[bx2])
                    P.op("vector", lambda e, half=half: e.tensor_tensor(ha[:, half, 0:NC], x2[:, 0:NC], xb[:, 0:NC], ALU.mult), reads=[bx2, bxb], writes=[bha])
                if j == 0:
                    for half in range(2):
                        P.op("tensor", lambda e, half=half: e.matmul(ps[5][0:64, 0:NCT * 128], W2b[:, half, :], ha[:, half, 0:NCT * 128], start=(half == 0), stop=(half == 1)),
                             reads=[bW2, bha], writes=[bps[5]], inc=(half == 1))
                    P.op("vector", lambda e: e.tensor_copy(KC[:, :], ps[5][0:64, 0:NCT * 128]), reads=[bps[5]], writes=[bKC])
                else:
                    for nt in range(NCT):
                        for half in range(2):
                            P.op("tensor", lambda e, half=half, nt=nt: e.matmul(ps[6][:, 0:64], ha[:, half, nt * 128:(nt + 1) * 128], W2b[:, half, :], start=(half == 0), stop=(half == 1)),
                                 reads=[bW2, bha], writes=[bps[6]], inc=(half == 1))
                        P.op("vector", lambda e, nt=nt: e.tensor_copy(Vcp[:, nt, 0:64], ps[6][:, 0:64]), reads=[bps[6]], writes=[bVcp])
            for qt in range(NQ):
                a = qt % 2
                qs = slice(qt * 128, (qt + 1) * 128)
                needB = qt >= 32
                for r in range(3):
                    hh = g_ * 3 + r
                    P.load("sync", lambda e, a=a, r=r, hh=hh, qs=qs: e.dma_start(out=QXA[a][0:64, r * 128:(r + 1) * 128], in_=g.FM[hh * 64:(hh + 1) * 64, qs]), bQXAq[a])
                P.load("gpsimd", lambda e, a=a, qs=qs: e.dma_start(out=gts[a][:], in_=g.GT[qs, :]), bgts[a])
                P.op("scalar", lambda e, a=a: e.activation(out=gts[a][:], in_=gts[a][:], func=AF.Sigmoid), reads=[bgts[a]], writes=[bgts[a]])
                nts = [nt for nt in range(NCT) if qt - 16 * nt >= 0]
                for ii, nt in enumerate(nts):
                    dlt = qt - 16 * nt
                    masks = []
                    if dlt <= 16:
                        for r in range(3):
                            masks.append((lambda e, pb, r=r, dlt=dlt: e.matmul(pb[:, r * 128:(r + 1) * 128], g.cb[:, 0, :], CM[:, 128 * dlt:128 * dlt + 128],
                                                                             start=False, stop=(r == 2), skip_group_check=True), [g.bcb, bconst]))
                    pt = attn_tile(lambda nt=nt: KC[0:64, nt * 128:(nt + 1) * 128], [bKC], 64, QXA[a], [bQXAq[a]], Vcp[:, nt, :], [bVcp, bV1], nt,
                                   3, 0, ii == 0, ii == len(nts) - 1, masks)
                    for r in range(3):
                        P.op("tensor", lambda e, r=r, pt=pt, nt=nt, ii=ii: e.matmul(ps[2][:, r * 128:(r + 1) * 128], PT[pt][:, r * 128:(r + 1) * 128], CMAP[:, nt, :],
                                                                                 start=(ii == 0 and r == 0), stop=(ii == len(nts) - 1), skip_group_check=True),
                             reads=[bPT[pt], bconst], writes=[bps[2]], inc=(r == 2))
                P.op("vector", lambda e: e.tensor_scalar(rlc[:], ps[3][:, 0:195].rearrange("p (r c) -> p r c", c=65)[:, :, 64], 1e-30, None, ALU.max),
                     reads=[bps[3]], writes=[brlc])
                P.op("vector", lambda e: e.reciprocal(rlc[:], rlc[:]), reads=[brlc], writes=[brlc])
                P.op("vector", lambda e: e.tensor_scalar(imp[:], ps[2][:, 0:128], rlc[:, 0:1], None, ALU.mult), reads=[bps[2], brlc], writes=[bimp])
                P.op("vector", lambda e: e.scalar_tensor_tensor(imp[:], ps[2][:, 128:256], rlc[:, 1:2], imp[:], ALU.mult, ALU.add), reads=[bps[2], brlc, bimp], writes=[bimp])
                P.op("vector", lambda e: e.scalar_tensor_tensor(imp[:], ps[2][:, 256:384], rlc[:, 2:3], imp[:], ALU.mult, ALU.add), reads=[bps[2], brlc, bimp], writes=[bimp])
                P.op("vector", lambda e, qt=qt: e.tensor_tensor(imp[:], imp[:], FBW[:, 128 - 2 * qt:256 - 2 * qt], ALU.add), reads=[bimp, bconst], writes=[bimp])
                P.op("vector", lambda e: e.memset(imp[:, 0:1], 1e9), writes=[bimp])
                P.op("vector", lambda e: e.max(out=m8a[:], in_=imp[:]), reads=[bimp], writes=[bm8a])
                P.op("vector", lambda e: e.match_replace(out=imp3[:], in_to_replace=m8a[:], in_values=imp[:], imm_value=-3e9), reads=[bimp, bm8a], writes=[bimp3])
                P.op("vector", lambda e: e.max(out=m8b[:], in_=imp3[:]), reads=[bimp3], writes=[bm8b])
                P.op("vector", lambda e: e.tensor_scalar(W192[:, 64:192], imp[:], m8b[:, 7:8], MNEG, ALU.is_lt, ALU.mult), reads=[bimp, bm8b], writes=[bW192])
                P.op("tensor", lambda e: e.transpose(ptb[:, 0:128], W192[:, 0:128], g.cb[:, 0, :]), reads=[bW192, g.bcb], writes=[bps[7]])
                if needB:
                    P.op("vector", lambda e: e.tensor_scalar(WB[:, 64:128], imp[:, 64:128], m8b[:, 7:8], MNEG, ALU.is_lt, ALU.mult), reads=[bimp, bm8b], writes=[bWB])
                for r in range(3):
                    P.op("scalar" if r != 1 else "vector",
                         (lambda e, a=a, r=r: e.activation(out=QXA[a][64:128, r * 128:(r + 1) * 128], in_=ptb[64:128, 0:128], func=AF.Copy)) if r != 1 else
                         (lambda e, a=a, r=r: e.tensor_copy(QXA[a][64:128, r * 128:(r + 1) * 128], ptb[64:128, 0:128])),
                         reads=[bps[7]], writes=[bQXAm[a]])
                wk = list(range(max(0, qt - 4), qt + 1))
                for ii, kt in enumerate(wk):
                    masks = []
                    if kt == qt:
                        masks.append((lambda e, pb: e.matmul(pb[:, 0:384], g.cb[:, 0, :], TRI[:, 0, :], start=False, stop=True, skip_group_check=True), [g.bcb, bconst]))
                    if kt == qt - 4:
                        masks.append((lambda e, pb: e.matmul(pb[:, 0:384], g.cb[:, 0, :], TRI[:, 1, :], start=False, stop=True, skip_group_check=True), [g.bcb, bconst]))
                    attn_tile(lambda kt=kt: KW[0:64, kt * 128:(kt + 1) * 128], [bKW], 64, QXA[a], [bQXAq[a]], VWp[:, kt, :], [bVW, bV1], kt,
                              3, 195, False, ii == len(wk) - 1, masks)
                for kt in range(qt + 1):
                    masks = []
                    if kt == qt:
                        masks.append((lambda e, pb: e.matmul(pb[:, 0:384], g.cb[:, 0, :], TRI[:, 0, :], start=False, stop=True, skip_group_check=True), [g.bcb, bconst]))
                    if kt == 32:
                        P.op("tensor", lambda e: e.transpose(ptb[:, 0:128], WB[:, 0:128], g.cb[:, 0, :]), reads=[bWB, g.bcb], writes=[bps[7]])
                        for r in range(3):
                            P.op("scalar" if r != 1 else "vector",
                                 (lambda e, a=a, r=r: e.activation(out=QXA[a][64:128, r * 128:(r + 1) * 128], in_=ptb[64:128, 0:128], func=AF.Copy)) if r != 1 else
                                 (lambda e, a=a, r=r: e.tensor_copy(QXA[a][64:128, r * 128:(r + 1) * 128], ptb[64:128, 0:128])),
                                 reads=[bps[7]], writes=[bQXAm[a]])
                    attn_tile(lambda kt=kt: KSX[:, kt * 128:(kt + 1) * 128], [bKSk, bKSo], 128, QXA[a],
                              [bQXAq[a], bQXAm[a]], VSp[:, kt, :], [bVS, bV1], kt,
                              4, 0, kt == 0, kt == qt, masks)
                pv3 = lambda bank, c0: ps[bank][:, c0:c0 + 195].rearrange("p (r c) -> p r c", c=65)
                P.op("vector", lambda e, a=a: e.tensor_scalar(L9[a][:, 0:3], pv3(3, 0)[:, :, 64], 1e-30, None, ALU.max), reads=[bps[3]], writes=[bL9[a]])
                P.op("vector", lambda e, a=a: e.tensor_scalar(L9[a][:, 3:6], pv3(4, 0)[:, :, 64], 1e-30, None, ALU.max), reads=[bps[4]], writes=[bL9[a]])
                P.op("vector", lambda e, a=a: e.tensor_scalar(L9[a][:, 6:9], pv3(3, 195)[:, :, 64], 1e-30, None, ALU.max), reads=[bps[3]], writes=[bL9[a]])
                P.op("vector", lambda e, a=a: e.reciprocal(L9[a][:], L9[a][:]), reads=[bL9[a]], writes=[bL9[a]])
                P.op("vector", lambda e, a=a, g_=g_: e.tensor_tensor(C9[a][:].rearrange("p (b r) -> p b r", r=3), L9[a][:].rearrange("p (b r) -> p b r", r=3),
                                                                  gts[a][:, g_ * 9:(g_ + 1) * 9].rearrange("p (r b) -> p b r", b=3), ALU.mult),
                     reads=[bL9[a], bgts[a]], writes=[bC9[a]])
                for r in range(3):
                    f = r % 2
                    hh = g_ * 3 + r
                    P.op("vector", lambda e, f=f, r=r, a=a: e.tensor_scalar(o32[f][:], ps[3][:, r * 65:r * 65 + 64], C9[a][:, r:r + 1], None, ALU.mult),
                         reads=[bps[3], bC9[a]], writes=[bo32[f]])
                    P.op("vector", lambda e, f=f, r=r, a=a: e.scalar_tensor_tensor(o32[f][:], ps[4][:, r * 65:r * 65 + 64], C9[a][:, 3 + r:4 + r], o32[f][:], ALU.mult, ALU.add),
                         reads=[bps[4], bC9[a], bo32[f]], writes=[bo32[f]])
                    P.op("vector", lambda e, f=f, r=r, a=a: e.scalar_tensor_tensor(o32[f][:], ps[3][:, 195 + r * 65:195 + r * 65 + 64], C9[a][:, 6 + r:7 + r], o32[f][:], ALU.mult, ALU.add),
                         reads=[bps[3], bC9[a], bo32[f]], writes=[bo32[f]])
                    P.op("scalar", lambda e, f=f: e.activation(out=junk[:], in_=o32[f][:], func=AF.Square, accum_out=fs[f][:, 1:2]),
                         reads=[bo32[f]], writes=[bjunk, bfs[f]])
                    P.op("scalar", lambda e, f=f: e.activation(out=fs[f][:, 2:3], in_=fs[f][:, 1:2], func=AF.Sqrt, scale=1.0 / 64, bias=NORM_EPS),
                         reads=[bfs[f]], writes=[bfs[f]])
                    P.op("vector", lambda e, f=f: e.reciprocal(fs[f][:, 3:4], fs[f][:, 2:3]), reads=[bfs[f]], writes=[bfs[f]])
                    P.op("vector", lambda e, f=f, hh=hh: e.scalar_tensor_tensor(ob[f][:], o32[f][:], fs[f][:, 3:4], gainb[:, hh * 64:(hh + 1) * 64], ALU.mult, ALU.mult),
                         reads=[bo32[f], bfs[f], bconst], writes=[bob[f]])
                    P.op("tensor", lambda e, f=f, r=r: e.transpose(ptb[0:64, 256 + r * 128:256 + (r + 1) * 128], ob[f][:], g.cb[:, 0, :]),
                         reads=[bob[f], g.bcb], writes=[bps[7]])
                P.op("scalar", lambda e, a=a: e.activation(out=OT[a][:].rearrange("p r t -> p (r t)"), in_=ptb[0:64, 256:640], func=AF.Copy), reads=[bps[7]], writes=[bOT[a]])
                for r in range(3):
                    P.store("gpsimd", lambda e, a=a, g_=g_, qs=qs, r=r: e.dma_start(out=g.OMT[g_ * 192 + r * 64:g_ * 192 + (r + 1) * 64, qs], in_=OT[a][:, r, :]), bOT[a])
        P.end_stage()


def host_consts_rw():
    p = np.arange(64)[:, None]
    f = np.arange(64)[None, :]
    ls = (p > f).astype(np.float32)
    us = (p < f).astype(np.float32)
    ui = (p <= f).astype(np.float32)
    return {"c_m5": np.ascontiguousarray(np.concatenate([ls, us, us, ui, ui], 1))}


def stage_rwkv(g, mu_ap, w0_ap, w2_ap, a0_ap, a2_ap, g2_ap, kk_ap, ka_ap, rk_ap, lnw_ap, lnb_ap, c_m5):
    nc, P, S = g.nc, g.P, g.S
    NT, C = 512, 64
    NCK = NT // C
    ps, bps = g.ps, g.bps
    B = P.buf
    I64 = g.cf[0:64, 0, 0:64]
    ONES = g.cf[0:64, 1, 0:64]
    with ExitStack() as st:
        sb = lambda name, shape, dt=F32: _sb(nc, st, "rw" + name, shape, dt)
        P12 = sb("P12", [64, 12, NT + 1]); SH = sb("SH", [64, 12, NT])
        wx = sb("wx", [32, NT + 1]); ax = sb("ax", [32, NT + 1]); gx = sb("gx", [64, NT + 1])
        shw = sb("shw", [32, NT]); sha = sb("sha", [32, NT]); shg = sb("shg", [64, NT])
        AT = sb("AT", [64, 4, NT]); BT = sb("BT", [64, 4, NT]); KT = sb("KT", [64, 4, NT]); RT = sb("RT", [64, 4, NT])
        GG = sb("GG", [64, 4, NT]); RK = sb("RK", [64, 4, NT]); gC = sb("gC", [64, 4, NCK])
        OUTT = sb("OUTT", [64, 4, NT], BF16)
        t1 = sb("t1", [64, NT]); t2 = sb("t2", [64, NT]); t3 = sb("t3", [64, NT]); cs = sb("cs", [64, NT]); lnw = sb("lnw", [64, NT]); aa = sb("aa", [64, NT])
        mu12 = sb("mu12", [64, 12]); muw = sb("muw", [32, 1]); mua = sb("mua", [32, 1]); mug = sb("mug", [64, 1])
        w0s = sb("w0s", [64, 4]); a0s = sb("a0s", [64, 4]); kks = sb("kks", [64, 4]); kas = sb("kas", [64, 4]); omk = sb("omk", [64, 4]); rks = sb("rks", [64, 4])
        w2s = sb("w2s", [32, 256]); a2s = sb("a2s", [32, 256]); g2s = sb("g2s", [64, 256])
        lnwb = sb("lnwb", [64, 256]); lnbb = sb("lnbb", [64, 256])
        M5c = sb("M5c", [64, 320])
        H = sb("H", [64, 4, 64]); Hs = sb("Hs", [64, 4, 64])
        M5 = [sb("M5_%d" % h, [64, 320]) for h in range(4)]
        PQ = [[sb("PQ%d_%d" % (h, i), [64, 128]) for i in range(2)] for h in range(4)]
        TT = [sb("TT%d" % h, [64, 64]) for h in range(4)]
        VBK = [sb("VBK%d" % h, [64, 192]) for h in range(4)]
        Xs = [sb("Xs%d" % h, [64, 64]) for h in range(4)]
        Us = [sb("Us%d" % h, [64, 64]) for h in range(4)]
        cen = [sb("cen%d" % h, [64, 64]) for h in range(4)]
        yy = [sb("yy%d" % h, [64, 64]) for h in range(4)]
        sst = [sb("sst%d" % h, [64, 6]) for h in range(4)]
        junk = sb("junk", [64, 64])
        bP12, bSH, bx3, bsh3, bprm, bt1, bt2, bt3, bcs, blnw, baa, bjunk, bOUT = (B("z") for _ in range(13))
        bAT, bBT, bKT, bRT, bGG, bRK, bgC = ([B("h%d" % h) for h in range(4)] for _ in range(7))
        bH = [B("H%d" % h) for h in range(4)]
        bHs = [B("Hs%d" % h) for h in range(4)]
        bM5 = [B("M5%d" % h) for h in range(4)]
        bPQ = [[B("PQ") for i in range(2)] for h in range(4)]
        bTT, bVBK, bXs, bUs, bcen, byy, bsst = ([B("q%d" % h) for h in range(4)] for _ in range(7))

        ld = lambda dst, src, bf, small=True: P.load("sync", lambda e: e.dma_start(out=dst, in_=src, allow_slow_non_contiguous=small), bf)
        ld(mu12[:], mu_ap[0:768].rearrange("(a p) -> p a", p=64), bprm)
        ld(muw[:], mu_ap[768:800].rearrange("(p o) -> p o", o=1), bprm)
        ld(mua[:], mu_ap[800:832].rearrange("(p o) -> p o", o=1), bprm)
        ld(mug[:], mu_ap[832:896].rearrange("(p o) -> p o", o=1), bprm)
        ld(w0s[:], w0_ap.rearrange("(h p) -> p h", p=64), bprm)
        ld(a0s[:], a0_ap.rearrange("(h p) -> p h", p=64), bprm)
        ld(kks[:], kk_ap.rearrange("(h p) -> p h", p=64), bprm)
        ld(kas[:], ka_ap.rearrange("(h p) -> p h", p=64), bprm)
        ld(rks[:], rk_ap.rearrange("h p -> p h"), bprm)
        ld(w2s[:], w2_ap[:, :], bprm)
        ld(a2s[:], a2_ap[:, :], bprm)
        ld(g2s[:], g2_ap[:, :], bprm)
        ld(lnwb[:], lnw_ap.partition_broadcast(64), bprm)
        ld(lnbb[:], lnb_ap.partition_broadcast(64), bprm)
        ld(M5c[:], c_m5[:, :], bprm)
        P.op("vector", lambda e: e.tensor_scalar(omk[:], kas[:], -1.0, 1.0, ALU.mult, ALU.add), reads=[bprm], writes=[bprm])
        for h in range(4):
            P.op("vector", lambda e, h=h: e.memset(H[:, h, :], 0.0), writes=[bH[h]])
        rwv = g.RW[0:768, :].rearrange("(a p) t -> p a t", p=64)

        for tt in range(S // NT):
            t0 = tt * NT
            if tt == 0:
                P.op("vector", lambda e: e.memset(P12[:, :, 0:1], 0.0), writes=[bP12])
                P.op("vector", lambda e: e.memset(wx[:, 0:1], 0.0), writes=[bx3])
                P.op("vector", lambda e: e.memset(ax[:, 0:1], 0.0), writes=[bx3])
                P.op("vector", lambda e: e.memset(gx[:, 0:1], 0.0), writes=[bx3])
                lo, c0 = 1, 0
            else:
                lo, c0 = 0, t0 - 1
            for i in range(12):
                P.load("sync" if i % 2 == 0 else "gpsimd", lambda e, lo=lo, c0=c0, t0=t0, i=i: e.dma_start(out=P12[:, i, lo:NT + 1], in_=g.RW[i * 64:(i + 1) * 64, c0:t0 + NT]), bP12)
            P.load("gpsimd", lambda e, lo=lo, c0=c0, t0=t0: e.dma_start(out=wx[:, lo:NT + 1], in_=g.RW[768:800, c0:t0 + NT]), bx3)
            P.load("gpsimd", lambda e, lo=lo, c0=c0, t0=t0: e.dma_start(out=ax[:, lo:NT + 1], in_=g.RW[800:832, c0:t0 + NT]), bx3)
            P.load("gpsimd", lambda e, lo=lo, c0=c0, t0=t0: e.dma_start(out=gx[:, lo:NT + 1], in_=g.RW[832:896, c0:t0 + NT]), bx3)
            for i in range(12):
                P.op("vector", lambda e, i=i: e.tensor_tensor(SH[:, i, :], P12[:, i, 0:NT], P12[:, i, 1:NT + 1], ALU.subtract), reads=[bP12], writes=[bSH])
                P.op("vector", lambda e, i=i: e.scalar_tensor_tensor(SH[:, i, :], SH[:, i, :], mu12[:, i:i + 1], P12[:, i, 1:NT + 1], ALU.mult, ALU.add),
                     reads=[bP12, bSH, bprm], writes=[bSH])
            for (xx, ss, mm) in ((wx, shw, muw), (ax, sha, mua), (gx, shg, mug)):
                P.op("vector", lambda e, xx=xx, ss=ss: e.tensor_tensor(ss[:], xx[:, 0:NT], xx[:, 1:NT + 1], ALU.subtract), reads=[bx3], writes=[bsh3])
                P.op("vector", lambda e, xx=xx, ss=ss, mm=mm: e.scalar_tensor_tensor(ss[:], ss[:], mm[:, 0:1], xx[:, 1:NT + 1], ALU.mult, ALU.add),
                     reads=[bx3, bsh3, bprm], writes=[bsh3])
            P.op("scalar", lambda e: e.activation(out=shw[:], in_=shw[:], func=AF.Tanh), reads=[bsh3], writes=[bsh3])
            P.op("scalar", lambda e: e.activation(out=shg[:], in_=shg[:], func=AF.Sigmoid), reads=[bsh3], writes=[bsh3])
            for h in range(4):
                hs_ = slice(h * 64, (h + 1) * 64)
                r_, k_, v_ = SH[:, h, :], SH[:, 4 + h, :], SH[:, 8 + h, :]
                P.op("tensor", lambda e, hs_=hs_: e.matmul(ps[0][0:64, 0:NT], w2s[:, hs_], shw[:], start=True, stop=True), reads=[bprm, bsh3], writes=[bps[0]])
                P.op("tensor", lambda e, hs_=hs_: e.matmul(ps[1][0:64, 0:NT], a2s[:, hs_], sha[:], start=True, stop=True), reads=[bprm, bsh3], writes=[bps[1]])
                P.op("tensor", lambda e, hs_=hs_: e.matmul(ps[2][0:64, 0:NT], g2s[:, hs_], shg[:], start=True, stop=True), reads=[bprm, bsh3], writes=[bps[2]])
                P.op("scalar", lambda e, h=h: e.activation(out=lnw[:], in_=ps[0][0:64, 0:NT], func=AF.Sigmoid, bias=w0s[:, h:h + 1]), reads=[bps[0], bprm], writes=[blnw])
                P.op("vector", lambda e: e.tensor_scalar(lnw[:], lnw[:], -0.606531, None, ALU.mult), reads=[blnw], writes=[blnw])
                P.op("scalar", lambda e, h=h: e.activation(out=aa[:], in_=ps[1][0:64, 0:NT], func=AF.Sigmoid, bias=a0s[:, h:h + 1]), reads=[bps[1], bprm], writes=[baa])
                P.op("scalar", lambda e, h=h: e.activation(out=GG[:, h, :], in_=ps[2][0:64, 0:NT], func=AF.Copy), reads=[bps[2]], writes=[bGG[h]])
                P.op("vector", lambda e, h=h, k_=k_: e.tensor_scalar(t1[:], k_, kks[:, h:h + 1], None, ALU.mult), reads=[bSH, bprm], writes=[bt1])
                P.op("scalar", lambda e: e.activation(out=t2[:], in_=t1[:], func=AF.Square), reads=[bt1], writes=[bt2])
                P.op("tensor", lambda e: e.matmul(ps[3][0:64, 0:NT], ONES, t2[:], start=True, stop=True), reads=[g.bcf, bt2], writes=[bps[3]])
                P.op("scalar", lambda e: e.activation(out=t2[:], in_=ps[3][0:64, 0:NT], func=AF.Sqrt), reads=[bps[3]], writes=[bt2])
                P.op("vector", lambda e: e.tensor_scalar(t2[:], t2[:], 1e-12, None, ALU.max), reads=[bt2], writes=[bt2])
                P.op("vector", lambda e: e.reciprocal(t2[:], t2[:]), reads=[bt2], writes=[bt2])
                P.op("vector", lambda e: e.tensor_tensor(t1[:], t1[:], t2[:], ALU.mult), reads=[bt1, bt2], writes=[bt1])
                P.op("vector", lambda e, h=h: e.tensor_scalar(t3[:], aa[:], kas[:, h:h + 1], omk[:, h:h + 1], ALU.mult, ALU.add), reads=[baa, bprm], writes=[bt3])
                P.op("vector", lambda e, k_=k_: e.tensor_tensor(t3[:], t3[:], k_, ALU.mult), reads=[bt3, bSH], writes=[bt3])
                P.op("vector", lambda e, h=h, r_=r_: e.scalar_tensor_tensor(RK[:, h, :], r_, rks[:, h:h + 1], t3[:], ALU.mult, ALU.mult),
                     reads=[bSH, bprm, bt3], writes=[bRK[h]])
                for c in range(NCK):
                    cc = slice(c * C, (c + 1) * C)
                    P.op("vector", lambda e, cc=cc: e.tensor_tensor_scan(cs[:, cc], g.cf[0:64, 1, 0:C], lnw[:, cc], 0.0, ALU.mult, ALU.add),
                         reads=[blnw, g.bcf], writes=[bcs])
                P.op("scalar", lambda e, h=h, r_=r_: e.activation(out=t2[:], in_=cs[:], func=AF.Exp), reads=[bcs], writes=[bt2])
                P.op("vector", lambda e, h=h, r_=r_: e.tensor_tensor(RT[:, h, :], r_, t2[:], ALU.mult), reads=[bSH, bt2], writes=[bRT[h]])
                P.op("vector", lambda e, h=h: e.tensor_copy(gC[:, h, :], t2[:, C - 1:NT:C]), reads=[bt2], writes=[bgC[h]])
                P.op("scalar", lambda e: e.activation(out=t2[:], in_=cs[:], func=AF.Exp, scale=-1.0), reads=[bcs], writes=[bt2])
                P.op("vector", lambda e, h=h: e.tensor_tensor(KT[:, h, :], t3[:], t2[:], ALU.mult), reads=[bt3, bt2], writes=[bKT[h]])
                P.op("vector", lambda e: e.tensor_tensor(t3[:], t1[:], aa[:], ALU.mult), reads=[bt1, baa], writes=[bt3])
                P.op("vector", lambda e, h=h: e.tensor_tensor(BT[:, h, :], t3[:], t2[:], ALU.mult), reads=[bt3, bt2], writes=[bBT[h]])
                P.op("vector", lambda e: e.tensor_tensor(t2[:], cs[:], lnw[:], ALU.subtract), reads=[bcs, blnw], writes=[bt2])
                P.op("scalar", lambda e: e.activation(out=t2[:], in_=t2[:], func=AF.Exp), reads=[bt2], writes=[bt2])
                P.op("vector", lambda e, h=h: e.scalar_tensor_tensor(AT[:, h, :], t1[:], -1.0, t2[:], ALU.mult, ALU.mult), reads=[bt1, bt2], writes=[bAT[h]])
            HB = lambda h: 4 + h
            for c in range(NCK):
                cc = slice(c * C, (c + 1) * C)
                for h in range(4):
                    A_, B_, K_, R_ = AT[:, h, cc], BT[:, h, cc], KT[:, h, cc], RT[:, h, cc]
                    rd = [bAT[h], bBT[h], bKT[h], bRT[h]]
                    for i, (l_, r2) in enumerate(((A_, B_), (B_, A_), (K_, A_), (B_, R_), (K_, R_))):
                        P.op("tensor", lambda e, h=h, i=i, l_=l_, r2=r2: e.matmul(ps[h][0:64, i * 64:(i + 1) * 64], l_, r2, start=(i == 0), stop=True, skip_group_check=True),
                             reads=rd, writes=[bps[h]], inc=(i == 4))
                    P.op("vector", lambda e, h=h: e.tensor_tensor(M5[h][:], ps[h][0:64, 0:320], M5c[:], ALU.mult), reads=[bps[h], bprm], writes=[bM5[h]])
                    for i, src in enumerate((SH[:, 8 + h, cc], B_, K_)):
                        P.op("tensor", lambda e, h=h, i=i, src=src: e.transpose(ps[HB(h)][0:64, 256 + i * 64:256 + (i + 1) * 64], src, I64),
                             reads=[bSH, bBT[h], bKT[h], g.bcf], writes=[bps[HB(h)]], inc=(i == 2))
                    P.op("scalar", lambda e, h=h: e.activation(out=VBK[h][:], in_=ps[HB(h)][0:64, 256:448], func=AF.Copy), reads=[bps[HB(h)]], writes=[bVBK[h]])
                    P.op("vector", lambda e, h=h: e.tensor_tensor(TT[h][:], M5[h][:, 64:128], I64, ALU.add), reads=[bM5[h], g.bcf], writes=[bTT[h]])
                    P.op("vector", lambda e, h=h, c=c: e.tensor_scalar(Hs[:, h, :], H[:, h, :], gC[:, h, c:c + 1], None, ALU.mult), reads=[bH[h], bgC[h]], writes=[bHs[h]])
                cur = [(M5[h], bM5[h]) for h in range(4)]
                for it in range(5):
                    for h in range(4):
                        ct, cb_ = cur[h]
                        P.op("tensor", lambda e, h=h, ct=ct: e.matmul(ps[h][0:64, 320:384], ct[:, 64:128], ct[:, 0:64], start=True, stop=True, skip_group_check=True),
                             reads=[cb_], writes=[bps[h]], inc=(it == 4))
                        if it < 4:
                            P.op("tensor", lambda e, h=h, ct=ct: e.matmul(ps[h][0:64, 384:448], ct[:, 0:64], ct[:, 64:128], start=False, stop=True, skip_group_check=True),
                                 reads=[cb_], writes=[bps[h]])
                    for h in range(4):
                        nt_, nb_ = PQ[h][it % 2], bPQ[h][it % 2]
                        P.op("scalar", lambda e, h=h, nt_=nt_: e.activation(out=nt_[:], in_=ps[h][0:64, 320:448], func=AF.Copy), reads=[bps[h]], writes=[nb_])
                        cur[h] = (nt_, nb_)
                    for h in range(4):
                        ct, cb_ = cur[h]
                        P.op("tensor", lambda e, h=h, ct=ct: e.matmul(ps[h][0:64, 448:512], ct[:, 0:64], TT[h][:], start=True, stop=True, skip_group_check=True),
                             reads=[cb_, bTT[h]], writes=[bps[h]])
                    for h in range(4):
                        P.op("vector", lambda e, h=h: e.tensor_tensor(TT[h][:], ps[h][0:64, 448:512], TT[h][:], ALU.add), reads=[bps[h], bTT[h]], writes=[bTT[h]])
                for h in range(4):
                    A_, R_ = AT[:, h, cc], RT[:, h, cc]
                    P.op("tensor", lambda e, h=h, A_=A_: e.matmul(ps[HB(h)][0:64, 0:64], A_, H[:, h, :], start=True, stop=False, skip_group_check=True),
                         reads=[bAT[h], bH[h]], writes=[bps[HB(h)]], inc=False)
                    P.op("tensor", lambda e, h=h: e.matmul(ps[HB(h)][0:64, 0:64], M5[h][:, 128:192], VBK[h][:, 0:64], start=False, stop=True, skip_group_check=True),
                         reads=[bM5[h], bVBK[h]], writes=[bps[HB(h)]])
                for h in range(4):
                    P.op("vector", lambda e, h=h: e.tensor_copy(Xs[h][:], ps[HB(h)][0:64, 0:64]), reads=[bps[HB(h)]], writes=[bXs[h]])
                for h in range(4):
                    P.op("tensor", lambda e, h=h: e.matmul(ps[HB(h)][0:64, 64:128], TT[h][:], Xs[h][:], start=True, stop=True, skip_group_check=True),
                         reads=[bTT[h], bXs[h]], writes=[bps[HB(h)]])
                for h in range(4):
                    P.op("scalar", lambda e, h=h: e.activation(out=Us[h][:], in_=ps[HB(h)][0:64, 64:128], func=AF.Copy), reads=[bps[HB(h)]], writes=[bUs[h]])
                for h in range(4):
                    R_ = RT[:, h, cc]
                    P.op("tensor", lambda e, h=h, R_=R_: e.matmul(ps[HB(h)][0:64, 128:192], R_, H[:, h, :], start=True, stop=False, skip_group_check=True),
                         reads=[bRT[h], bH[h]], writes=[bps[HB(h)]], inc=False)
                    P.op("tensor", lambda e, h=h: e.matmul(ps[HB(h)][0:64, 128:192], M5[h][:, 192:256], Us[h][:], start=False, stop=False, skip_group_check=True),
                         reads=[bM5[h], bUs[h]], writes=[bps[HB(h)]], inc=False)
                    P.op("tensor", lambda e, h=h: e.matmul(ps[HB(h)][0:64, 128:192], M5[h][:, 256:320], VBK[h][:, 0:64], start=False, stop=True, skip_group_check=True),
                         reads=[bM5[h], bVBK[h]], writes=[bps[HB(h)]], inc=False)
                    P.op("tensor", lambda e, h=h, cc=cc: e.matmul(ps[HB(h)][0:64, 192:193], RK[:, h, cc], g.cf[0:64, 1, 0:1], start=False, stop=True, skip_group_check=True),
                         reads=[bRK[h], g.bcf], writes=[bps[HB(h)]], inc=False)
                    P.op("tensor", lambda e, h=h: e.matmul(ps[HB(h)][0:64, 194:258], VBK[h][:, 64:128], Us[h][:], start=False, stop=False, skip_group_check=True),
                         reads=[bVBK[h], bUs[h]], writes=[bps[HB(h)]], inc=False)
                    P.op("tensor", lambda e, h=h: e.matmul(ps[HB(h)][0:64, 194:258], VBK[h][:, 128:192], VBK[h][:, 0:64], start=False, stop=True, skip_group_check=True),
                         reads=[bVBK[h]], writes=[bps[HB(h)]])
                for h in range(4):
                    pO = ps[HB(h)][0:64, 128:192]
                    P.op("vector", lambda e, h=h, c=c: e.scalar_tensor_tensor(H[:, h, :], ps[HB(h)][0:64, 194:258], gC[:, h, c:c + 1], Hs[:, h, :], ALU.mult, ALU.add),
                         reads=[bps[HB(h)], bgC[h], bHs[h]], writes=[bH[h]])
                    P.op("scalar", lambda e, h=h, pO=pO: e.activation(out=junk[:], in_=pO, func=AF.Identity, accum_out=sst[h][:, 0:1]), reads=[bps[HB(h)]], writes=[bjunk, bsst[h]])
                    P.op("vector", lambda e, h=h: e.tensor_scalar(sst[h][:, 1:2], sst[h][:, 0:1], 1.0 / 64, None, ALU.mult), reads=[bsst[h]], writes=[bsst[h]])
                    P.op("vector", lambda e, h=h, pO=pO: e.tensor_scalar(cen[h][:], pO, sst[h][:, 1:2], None, ALU.subtract), reads=[bps[HB(h)], bsst[h]], writes=[bcen[h]])
                    P.op("scalar", lambda e, h=h: e.activation(out=junk[:], in_=cen[h][:], func=AF.Square, accum_out=sst[h][:, 2:3]), reads=[bcen[h]], writes=[bjunk, bsst[h]])
                    P.op("scalar", lambda e, h=h: e.activation(out=sst[h][:, 3:4], in_=sst[h][:, 2:3], func=AF.Sqrt, scale=1.0 / 64, bias=64e-5), reads=[bsst[h]], writes=[bsst[h]])
                    P.op("vector", lambda e, h=h: e.reciprocal(sst[h][:, 4:5], sst[h][:, 3:4]), reads=[bsst[h]], writes=[bsst[h]])
                    P.op("vector", lambda e, h=h: e.scalar_tensor_tensor(yy[h][:], cen[h][:], sst[h][:, 4:5], lnwb[:, h * 64:(h + 1) * 64], ALU.mult, ALU.mult),
                         reads=[bcen[h], bsst[h], bprm], writes=[byy[h]])
                    P.op("vector", lambda e, h=h: e.tensor_tensor(yy[h][:], yy[h][:], lnbb[:, h * 64:(h + 1) * 64], ALU.add), reads=[byy[h], bprm], writes=[byy[h]])
                    P.op("vector", lambda e, h=h: e.tensor_copy(sst[h][:, 5:6], ps[HB(h)][0:64, 192:193]), reads=[bps[HB(h)]], writes=[bsst[h]])
                    P.op("vector", lambda e, h=h: e.scalar_tensor_tensor(yy[h][:], VBK[h][:, 0:64], sst[h][:, 5:6], yy[h][:], ALU.mult, ALU.add),
                         reads=[bVBK[h], bsst[h], byy[h]], writes=[byy[h]])
                    P.op("tensor", lambda e, h=h: e.transpose(ps[HB(h)][0:64, 448:512], yy[h][:], I64), reads=[byy[h], g.bcf], writes=[bps[HB(h)]])
                    P.op("vector", lambda e, h=h, cc=cc: e.tensor_tensor(OUTT[:, h, cc], ps[HB(h)][0:64, 448:512], GG[:, h, cc], ALU.mult), reads=[bps[HB(h)], bGG[h]], writes=[bOUT])
            for h in range(4):
                P.store("gpsimd", lambda e, t0=t0, h=h: e.dma_start(out=g.OMT[384 + h * 64:384 + (h + 1) * 64, t0:t0 + NT], in_=OUTT[:, h, :]), bOUT)
        P.end_stage()


_PARAM_SHAPES = {
    "attn_norm": [DEPTH, 1024], "w_in": [DEPTH, 1024, N_IN], "nsa_cmp_pos": [DEPTH, 2, 32, 64], "nsa_cmp_w1": [DEPTH, 2, 2048, 256],
    "nsa_cmp_w2": [DEPTH, 2, 256, 64], "nsa_out_gain": [DEPTH, 384], "rw_mu": [DEPTH, 896], "rw_w0": [DEPTH, 256], "rw_w2": [DEPTH, 32, 256],
    "rw_a0": [DEPTH, 256], "rw_a2": [DEPTH, 32, 256], "rw_g2": [DEPTH, 64, 256], "rw_k_k": [DEPTH, 256], "rw_k_a": [DEPTH, 256],
    "rw_r_k": [DEPTH, 4, 64], "rw_lnx_w": [DEPTH, 256], "rw_lnx_b": [DEPTH, 256], "moba_out_gain": [DEPTH, 384], "w_out": [DEPTH, 1024, 1024],
    "ffn_norm": [DEPTH, 1024], "ffn_w_in": [DEPTH, 1024, 2 * D_FF], "ffn_conv_w": [DEPTH, 3, D_FF], "ffn_conv_b": [DEPTH, D_FF],
    "ffn_w_out": [DEPTH, D_FF, 1024], "final_norm": [1024],
}


def all_host_consts(S):
    c = {}
    c.update(host_consts())
    c.update(host_consts_s(S))
    c.update(host_consts_nsa(S))
    c.update(host_consts_rw())
    return c


def build_full(S=SEQ, depth=DEPTH, stages="pnrm3f", debug=False):
    nc = bass.Bass("TRN2", target_bir_lowering=False)
    inp = lambda name, shape, dt=F32: nc.dram_tensor(name, list(shape), dt, kind="ExternalInput").ap()
    xT = inp("xT", [D_MODEL, S])
    A = {k: inp(k, v) for k, v in _PARAM_SHAPES.items()}
    hc = all_host_consts(S)
    CA = {k: inp(k, v.shape, F32 if v.dtype == np.float32 else BF16) for k, v in hc.items() if k not in ("c_cb", "c_cf")}
    outT = nc.dram_tensor("outT", [D_MODEL, S], F32, kind="ExternalOutput").ap()
    with ExitStack() as es:
        g = setup_globals(nc, es, S, debug=debug)
        load_consts(g)
        for l in range(depth):
            x_src = xT if l == 0 else g.X
            if "p" in stages:
                stage_p1(g, x_src, A["attn_norm"][l], A["w_in"][l])
            if "n" in stages:
              stage_nsa(g, A["nsa_out_gain"][l], A["nsa_cmp_pos"][l], A["nsa_cmp_w1"][l], A["nsa_cmp_w2"][l],
                      CA["c_ohs"], CA["c_cm"], CA["c_cmap"], CA["c_fbw"])
            if "r" in stages:
              stage_rwkv(g, A["rw_mu"][l], A["rw_w0"][l], A["rw_w2"][l], A["rw_a0"][l], A["rw_a2"][l], A["rw_g2"][l], A["rw_k_k"][l],
                       A["rw_k_a"][l], A["rw_r_k"][l], A["rw_lnx_w"][l], A["rw_lnx_b"][l], CA["c_m5"])
            if "m" in stages:
                stage_moba(g, A["moba_out_gain"][l], CA["c_ohm"])
            if "3" in stages:
                stage_p3a(g, x_src, A["w_out"][l])
                stage_p3b(g, A["ffn_norm"][l], A["ffn_w_in"][l], A["ffn_conv_w"][l], A["ffn_conv_b"][l], A["ffn_w_out"][l])
        if "f" in stages:
            stage_final(g, A["final_norm"], outT)
    return nc, g


def kernel(**inputs):
    x = np.asarray(inputs["x"], np.float32)
    Bn, S, D = x.shape
    nc, g = build_full(S)
    hc = all_host_consts(S)
    maps = []
    for b in range(Bn):
        m = {"xT": np.ascontiguousarray(x[b].T)}
        for k in _PARAM_SHAPES:
            m[k] = np.ascontiguousarray(np.asarray(inputs[k], np.float32))
        m.update(hc)
        maps.append(m)
    res = run_bass_kernel_spmd(nc, maps, core_ids=list(range(Bn)))
    out = np.stack([np.ascontiguousarray(np.asarray(res.results[b]["outT"], np.float32).T) for b in range(Bn)], 0)
    return out
```

```python
from contextlib import ExitStack
import numpy as np
import concourse.bass as bass
import concourse.mybir as mybir
from concourse.bass_utils import run_bass_kernel_spmd

F32 = mybir.dt.float32
BF16 = mybir.dt.bfloat16
AF = mybir.ActivationFunctionType
ALU = mybir.AluOpType
AX = mybir.AxisListType

D_MODEL = 1024
BATCH = 2
SEQ = 8192
DEPTH = 4
HD = 64
N_IN = 3218
N_IN_NSA = 1170
N_IN_RWKV = 896
N_IN_MOBA = 1152
D_FF = 2816
NORM_EPS = 1e-6
NCORES = 8
NEG = -1000.0


class Buf:
    __slots__ = ("name", "last_w", "readers", "dsem", "dcount")

    def __init__(self, name):
        self.name = name
        self.last_w = None
        self.readers = {}
        self.dsem = None
        self.dcount = 0


class Prog:
    ENGS = ("sync", "gpsimd", "scalar", "vector", "tensor")
    NDSEM = 48

    def __init__(self, nc, es):
        self.nc = nc
        self.es = es
        self.lists = {e: [] for e in self.ENGS}
        self.count = {e: 0 for e in self.ENGS}
        self.sem = {e: es.enter_context(nc.semaphore("s_" + e)) for e in self.ENGS}
        self.waited = {e: {} for e in self.ENGS}
        self.pool = [[es.enter_context(nc.semaphore("d%d" % i)), 0] for i in range(self.NDSEM)]
        self.free = list(range(self.NDSEM))
        self.stage_bufs = []
        self.stores = {}
        self.ninst = 0

    def buf(self, name):
        return Buf(name)

    def _dsem(self, b):
        if b.dsem is None:
            i = self.free.pop()
            b.dsem = self.pool[i][0]
            b.dcount = self.pool[i][1]
            self.stage_bufs.append((b, i))
        return b.dsem

    def _need(self, eng, deps):
        w = self.waited[eng]
        best = {}
        for sem, val in deps:
            k = id(sem)
            if w.get(k, 0) >= val:
                continue
            if k not in best or best[k][1] < val:
                best[k] = (sem, val)
        for k, (sem, val) in best.items():
            self.lists[eng].append(("wait", sem, val))
            w[k] = val

    @staticmethod
    def _deps(reads, writes):
        deps = []
        for b in reads:
            if b.last_w is not None:
                deps.append(b.last_w)
        for b in writes:
            if b.last_w is not None:
                deps.append(b.last_w)
            deps.extend(b.readers.values())
        return deps

    @staticmethod
    def _mark(tick, reads, writes):
        k = id(tick[0])
        for b in reads:
            if k not in b.readers or b.readers[k][1] < tick[1]:
                b.readers[k] = tick
        for b in writes:
            b.last_w = tick
            b.readers = {}

    def op(self, eng, fn, reads=(), writes=(), inc=True):
        deps = self._deps(reads, writes)
        own = self.sem[eng]
        if eng == "tensor":
            deps = [d for d in deps if d[0] is not own]
        self._need(eng, deps)
        self.ninst += 1
        if inc:
            self.count[eng] += 1
            tick = (own, self.count[eng])
            self.lists[eng].append(("op", fn, own, 1))
        else:
            assert eng == "tensor"
            tick = (own, self.count[eng] + 1)
            self.lists[eng].append(("op", fn, None, 0))
        self._mark(tick, reads, writes)
        return tick

    def load(self, eng, fn, dst, reads=()):
        self._need(eng, self._deps(reads, [dst]))
        sem = self._dsem(dst)
        dst.dcount += 16
        tick = (sem, dst.dcount)
        self.lists[eng].append(("op", fn, sem, 16))
        self.ninst += 1
        self._mark(tick, reads, [dst])
        return tick

    def store(self, eng, fn, src, writes=(), final=False):
        self._need(eng, self._deps([src], writes))
        sem = self._dsem(src)
        src.dcount += 16
        tick = (sem, src.dcount)
        self.lists[eng].append(("op", fn, sem, 16))
        self.ninst += 1
        self._mark(tick, [src], writes)
        self.stores[id(sem)] = tick
        return tick

    def barrier(self):
        deps = [(self.sem[e], self.count[e]) for e in self.ENGS if self.count[e] > 0]
        for b, i in self.stage_bufs:
            deps.append((b.dsem, b.dcount))
        for e in self.ENGS:
            own = self.sem[e]
            self._need(e, [d for d in deps if not (e == "tensor" and d[0] is own)])

    def end_stage(self):
        self.barrier()
        for b, i in self.stage_bufs:
            self.pool[i][1] = b.dcount
            self.free.append(i)
        self.stage_bufs = []
        self.stores = {}
        self.emit()

    def emit(self):
        lists = self.lists
        self.lists = {e: [] for e in self.ENGS}
        with self.nc.Block() as block:
            def run(eng_name):
                def body(e):
                    for it in lists[eng_name]:
                        if it[0] == "wait":
                            e.wait_ge(it[1], it[2])
                        elif it[2] is None:
                            it[1](e)
                        else:
                            it[1](e).then_inc(it[2], it[3])
                return body
            block.sync(run("sync"))
            block.gpsimd(run("gpsimd"))
            block.scalar(run("scalar"))
            block.vector(run("vector"))
            block.tensor(run("tensor"))


_UID = [0]


def _sb(nc, es, name, shape, dt):
    _UID[0] += 1
    return es.enter_context(nc.sbuf_tensor("%s_u%d" % (name, _UID[0]), list(shape), dt))


def _ps(nc, es, name, shape=(128, 512), dt=F32):
    return es.enter_context(nc.psum_tensor(name, list(shape), dt))


FM_COLS = [0, 128, 256, 384, 512, 640, 896] + [2066 + 128 * i for i in range(6)]
RW_COLS = [1170 + 128 * i for i in range(7)]
MNEG = -8000.0


class G:
    pass


def setup_globals(nc, es, S, debug=False):
    g = G()
    g.nc, g.es, g.S = nc, es, S
    g.P = Prog(nc, es)
    P = g.P
    g.NQ = S // 128
    d = lambda name, shape, dt: nc.dram_tensor(name, list(shape), dt, kind=("ExternalOutput" if debug else "Internal")).ap()
    g.FM = d("FM", [13 * 128, S], BF16)
    g.RW = d("RW", [896, S], F32)
    g.TMV = d("TMV", [S, 640], BF16)
    g.GT = d("GT", [S, 18], F32)
    g.OMT = d("OMT", [1024, S], BF16)
    g.X = d("X", [D_MODEL, S], F32)
    g.ps = [_ps(nc, es, "ps%d" % i) for i in range(8)]
    g.bps = [P.buf("ps%d" % i) for i in range(8)]
    g.cb = _sb(nc, es, "cb", [128, 4, 128], BF16)
    g.bcb = P.buf("cb")
    g.cf = _sb(nc, es, "cf", [128, 2, 128], F32)
    g.bcf = P.buf("cf")
    return g


def host_consts():
    import ml_dtypes
    i = np.arange(128)
    cb = np.zeros((128, 4, 128), np.float32)
    cb[:, 0, :] = np.eye(128)
    cb[:, 1, :] = np.where(i[:, None] <= i[None, :], 0.0, MNEG)
    cb[:, 2, :] = np.where(i[:, None] > i[None, :], 0.0, MNEG)
    cb[:, 3, :] = 1.0
    cf = np.zeros((128, 2, 128), np.float32)
    cf[:, 0, :] = np.eye(128)
    cf[:, 1, :] = 1.0
    return {"c_cb": cb.astype(ml_dtypes.bfloat16), "c_cf": cf}


def host_consts_s(S):
    import ml_dtypes
    key = np.arange(S)
    ohm = (key[None, :] // 256 == np.arange(32)[:, None]).astype(np.float32)
    return {"c_ohm": ohm.astype(ml_dtypes.bfloat16)}


def load_consts(g):
    nc, P = g.nc, g.P
    c_cb = nc.dram_tensor("c_cb", [128, 4, 128], BF16, kind="ExternalInput").ap()
    c_cf = nc.dram_tensor("c_cf", [128, 2, 128], F32, kind="ExternalInput").ap()
    P.load("sync", lambda e: e.dma_start(out=g.cb[:], in_=c_cb[:, :, :]), g.bcb)
    P.load("sync", lambda e: e.dma_start(out=g.cf[:], in_=c_cf[:, :, :]), g.bcf)


def stage_p1(g, x_src, gain_ap, w_ap):
    nc, P, S = g.nc, g.P, g.S
    KC = 8
    NT = 512
    with ExitStack() as st:
        W = _sb(nc, st, "p1W", [128, KC, N_IN], BF16)
        gs = _sb(nc, st, "p1gs", [128, KC], F32)
        stg = [_sb(nc, st, "p1stg%d" % i, [128, N_IN], F32) for i in range(2)]
        xs = [_sb(nc, st, "p1xs%d" % i, [128, KC, NT], F32) for i in range(2)]
        hs = [_sb(nc, st, "p1hs%d" % i, [128, KC, NT], BF16) for i in range(2)]
        sq = [_sb(nc, st, "p1sq%d" % i, [128, KC, NT], BF16) for i in range(2)]
        rb = [_sb(nc, st, "p1rb%d" % i, [128, NT], F32) for i in range(2)]
        rc = [_sb(nc, st, "p1rc%d" % i, [128, 4], F32) for i in range(2)]
        ofm = [_sb(nc, st, "p1ofm%d" % i, [128, NT], BF16) for i in range(4)]
        orw = [_sb(nc, st, "p1orw%d" % i, [128, NT], F32) for i in range(4)]
        otm = [_sb(nc, st, "p1otm%d" % i, [128, 640], BF16) for i in range(2)]
        ogt = [_sb(nc, st, "p1ogt%d" % i, [128, 18], F32) for i in range(2)]
        B = P.buf
        bW = [B("W%d" % k) for k in range(KC)]
        bgs = B("gs")
        bstg = [B("stg0"), B("stg1")]
        bxs, bhs, bsq, brb, brc = ([B("b%d" % i) for i in range(2)] for _ in range(5))
        bofm = [B("ofm%d" % i) for i in range(4)]
        borw = [B("orw%d" % i) for i in range(4)]
        botm = [B("otm%d" % i) for i in range(2)]
        bogt = [B("ogt%d" % i) for i in range(2)]
        ps, bps = g.ps, g.bps

        P.load("sync", lambda e: e.dma_start(out=gs[:], in_=gain_ap.rearrange("(k p) -> p k", p=128), allow_slow_non_contiguous=True), bgs)
        for k in range(KC):
            s = k % 2
            P.load("sync" if k % 2 == 0 else "gpsimd",
                   lambda e, k=k, s=s: e.dma_start(out=stg[s][:], in_=w_ap[k * 128:(k + 1) * 128, :]), bstg[s])
            P.op("vector" if k % 2 == 0 else "vector",
                 lambda e, k=k, s=s: e.tensor_scalar(W[:, k, :], stg[s][:], gs[:, k:k + 1], None, ALU.mult),
                 reads=[bstg[s], bgs], writes=[bW[k]])
        import os
        DBG = int(os.environ.get("DBG", "9"))
        xv = x_src.rearrange("(k p) t -> p k t", p=128)
        pi = 0
        oi = 0
        for t in range(S // NT if DBG >= 2 else 0):
            s = t % 2
            t0 = t * NT
            for k in range(KC):
                P.load("sync" if k % 2 == 0 else "gpsimd", lambda e, s=s, t0=t0, k=k: e.dma_start(out=xs[s][:, k, :], in_=x_src[k * 128:(k + 1) * 128, t0:t0 + NT]), bxs[s])
            P.op("scalar", lambda e, s=s: e.activation(out=sq[s][:], in_=xs[s][:], func=AF.Square), reads=[bxs[s]], writes=[bsq[s]])
            P.op("vector", lambda e, s=s: e.tensor_copy(hs[s][:], xs[s][:]), reads=[bxs[s]], writes=[bhs[s]])
            for k in range(KC):
                P.op("tensor", lambda e, k=k, s=s: e.matmul(ps[0][:, 0:NT], g.cb[:, 3, :], sq[s][:, k, :], start=(k == 0), stop=(k == KC - 1)),
                     reads=[bsq[s], g.bcb], writes=[bps[0]], inc=(k == KC - 1))
            for j in range(4):
                for k in range(KC):
                    P.op("tensor", lambda e, k=k, s=s, j=j: e.matmul(ps[1][:, j:j + 1], sq[s][:, k, j * 128:(j + 1) * 128], g.cb[:, 3, 0:1],
                                                                   start=(j == 0 and k == 0), stop=(k == KC - 1), skip_group_check=True),
                         reads=[bsq[s], g.bcb], writes=[bps[1]], inc=(j == 3 and k == KC - 1))
            P.op("scalar", lambda e, s=s: e.activation(out=rb[s][:], in_=ps[0][:, 0:NT], func=AF.Sqrt, scale=1.0 / D_MODEL, bias=NORM_EPS),
                 reads=[bps[0]], writes=[brb[s]])
            P.op("vector", lambda e, s=s: e.reciprocal(rb[s][:], rb[s][:]), reads=[brb[s]], writes=[brb[s]])
            P.op("scalar", lambda e, s=s: e.activation(out=rc[s][:], in_=ps[1][:, 0:4], func=AF.Sqrt, scale=1.0 / D_MODEL, bias=NORM_EPS),
                 reads=[bps[1]], writes=[brc[s]])
            P.op("vector", lambda e, s=s: e.reciprocal(rc[s][:], rc[s][:]), reads=[brc[s]], writes=[brc[s]])
            for ci, c0 in enumerate(FM_COLS + RW_COLS if DBG >= 3 else []):
                pb = 2 + pi % 6
                pi += 1
                for k in range(KC):
                    P.op("tensor", lambda e, k=k, s=s, pb=pb, c0=c0: e.matmul(ps[pb][:, 0:NT], W[:, k, c0:c0 + 128], hs[s][:, k, :], start=(k == 0), stop=(k == KC - 1)),
                         reads=[bhs[s], bW[k]], writes=[bps[pb]], inc=(k == KC - 1))
                o = oi % 4
                oi += 1
                if ci < 13:
                    P.op("vector", lambda e, s=s, pb=pb, o=o: e.tensor_tensor(ofm[o][:], ps[pb][:, 0:NT], rb[s][:], ALU.mult),
                         reads=[bps[pb], brb[s]], writes=[bofm[o]])
                    P.store("sync", lambda e, o=o, ci=ci, t0=t0: e.dma_start(out=g.FM[ci * 128:(ci + 1) * 128, t0:t0 + NT], in_=ofm[o][:]), bofm[o])
                else:
                    ri = ci - 13
                    P.op("vector", lambda e, s=s, pb=pb, o=o: e.tensor_tensor(orw[o][:], ps[pb][:, 0:NT], rb[s][:], ALU.mult),
                         reads=[bps[pb], brb[s]], writes=[borw[o]])
                    P.store("sync", lambda e, o=o, ri=ri, t0=t0: e.dma_start(out=g.RW[ri * 128:(ri + 1) * 128, t0:t0 + NT], in_=orw[o][:]), borw[o])
            for j in range(4 if DBG >= 4 else 0):
                o = j % 2
                pa = 2 + pi % 6
                pi += 1
                pb = 2 + pi % 6
                pi += 1
                for k in range(KC):
                    P.op("tensor", lambda e, k=k, s=s, j=j, pa=pa: e.matmul(ps[pa][:, 0:128], hs[s][:, k, j * 128:(j + 1) * 128], W[:, k, 768:896],
                                                                          start=(k == 0), stop=(k == KC - 1), skip_group_check=True),
                         reads=[bhs[s], bW[k]], writes=[bps[pa]], inc=False)
                for k in range(KC):
                    P.op("tensor", lambda e, k=k, s=s, j=j, pa=pa: e.matmul(ps[pa][:, 128:274], hs[s][:, k, j * 128:(j + 1) * 128], W[:, k, 1024:1170],
                                                                          start=False, stop=(k == KC - 1), skip_group_check=True),
                         reads=[bhs[s], bW[k]], writes=[bps[pa]], inc=(k == KC - 1))
                for k in range(KC):
                    P.op("tensor", lambda e, k=k, s=s, j=j, pb=pb: e.matmul(ps[pb][:, 0:384], hs[s][:, k, j * 128:(j + 1) * 128], W[:, k, 2834:3218],
                                                                          start=(k == 0), stop=(k == KC - 1)),
                         reads=[bhs[s], bW[k]], writes=[bps[pb]], inc=(k == KC - 1))
                P.op("scalar", lambda e, s=s, j=j, pa=pa, o=o: e.activation(out=otm[o][:, 0:256], in_=ps[pa][:, 0:256], func=AF.Copy, scale=rc[s][:, j:j + 1]),
                     reads=[bps[pa], brc[s]], writes=[botm[o]])
                P.op("scalar", lambda e, s=s, j=j, pa=pa, o=o: e.activation(out=ogt[o][:], in_=ps[pa][:, 256:274], func=AF.Copy, scale=rc[s][:, j:j + 1]),
                     reads=[bps[pa], brc[s]], writes=[bogt[o]])
                P.op("scalar", lambda e, s=s, j=j, pb=pb, o=o: e.activation(out=otm[o][:, 256:640], in_=ps[pb][:, 0:384], func=AF.Copy, scale=rc[s][:, j:j + 1]),
                     reads=[bps[pb], brc[s]], writes=[botm[o]])
                r0 = t0 + j * 128
                P.store("gpsimd", lambda e, o=o, r0=r0: e.dma_start(out=g.TMV[r0:r0 + 128, :], in_=otm[o][:]), botm[o])
                P.store("gpsimd", lambda e, o=o, r0=r0: e.dma_start(out=g.GT[r0:r0 + 128, :], in_=ogt[o][:]), bogt[o])
        P.end_stage()


def _load_cast_rows(P, nc, st, name, dst_tile, dst_bufs, src_ap, nrow_chunks, ncols, colblk, scale_ap=None, scale_buf=None):
    stg = [_sb(nc, st, "%s_stg%d" % (name, i), [128, colblk], F32) for i in range(2)]
    bstg = [P.buf("%s_stg%d" % (name, i)) for i in range(2)]
    i = 0
    for r in range(nrow_chunks):
        for c0 in range(0, ncols, colblk):
            c1 = min(ncols, c0 + colblk)
            s = i % 2
            i += 1
            P.load("sync" if s == 0 else "gpsimd",
                   lambda e, r=r, c0=c0, c1=c1, s=s: e.dma_start(out=stg[s][:, 0:c1 - c0], in_=src_ap[r * 128:(r + 1) * 128, c0:c1]), bstg[s])
            if scale_ap is None:
                if s == 0:
                    P.op("vector", lambda e, r=r, c0=c0, c1=c1, s=s: e.tensor_copy(dst_tile[:, r, c0:c1], stg[s][:, 0:c1 - c0]),
                         reads=[bstg[s]], writes=[dst_bufs[r]])
                else:
                    P.op("scalar", lambda e, r=r, c0=c0, c1=c1, s=s: e.activation(out=dst_tile[:, r, c0:c1], in_=stg[s][:, 0:c1 - c0], func=AF.Copy),
                         reads=[bstg[s]], writes=[dst_bufs[r]])
            else:
                P.op("vector", lambda e, r=r, c0=c0, c1=c1, s=s: e.tensor_scalar(dst_tile[:, r, c0:c1], stg[s][:, 0:c1 - c0], scale_ap[:, r:r + 1], None, ALU.mult),
                     reads=[bstg[s], scale_buf], writes=[dst_bufs[r]])


def stage_p3a(g, x_src, wout_ap):
    nc, P, S = g.nc, g.P, g.S
    KC, NT = 8, 512
    ps, bps = g.ps, g.bps
    with ExitStack() as st:
        Wo = _sb(nc, st, "p3Wo", [128, KC, D_MODEL], BF16)
        bWo = [P.buf("Wo%d" % k) for k in range(KC)]
        _load_cast_rows(P, nc, st, "p3a", Wo, bWo, wout_ap, KC, D_MODEL, D_MODEL)
        xt = [_sb(nc, st, "p3xt%d" % i, [128, KC, NT], F32) for i in range(2)]
        om = [_sb(nc, st, "p3om%d" % i, [128, KC, NT], BF16) for i in range(2)]
        bxt = [P.buf("xt%d" % i) for i in range(2)]
        bom = [P.buf("om%d" % i) for i in range(2)]
        xv = x_src.rearrange("(k p) t -> p k t", p=128)
        ov = g.OMT.rearrange("(k p) t -> p k t", p=128)
        xo = g.X.rearrange("(k p) t -> p k t", p=128)
        pi = 0
        for t in range(S // NT):
            s = t % 2
            t0 = t * NT
            for k in range(KC):
                P.load("sync", lambda e, s=s, t0=t0, k=k: e.dma_start(out=xt[s][:, k, :], in_=x_src[k * 128:(k + 1) * 128, t0:t0 + NT]), bxt[s])
                P.load("gpsimd", lambda e, s=s, t0=t0, k=k: e.dma_start(out=om[s][:, k, :], in_=g.OMT[k * 128:(k + 1) * 128, t0:t0 + NT]), bom[s])
            for m in range(KC):
                pb = pi % 8
                pi += 1
                for k in range(KC):
                    P.op("tensor", lambda e, k=k, m=m, s=s, pb=pb: e.matmul(ps[pb][:, 0:NT], Wo[:, k, m * 128:(m + 1) * 128], om[s][:, k, :], start=(k == 0), stop=(k == KC - 1)),
                         reads=[bom[s], bWo[k]], writes=[bps[pb]], inc=(k == KC - 1))
                P.op("vector", lambda e, m=m, s=s, pb=pb: e.tensor_tensor(xt[s][:, m, :], ps[pb][:, 0:NT], xt[s][:, m, :], ALU.add),
                     reads=[bps[pb], bxt[s]], writes=[bxt[s]])
            for k in range(KC):
                P.store("sync" if k % 2 == 0 else "gpsimd", lambda e, s=s, t0=t0, k=k: e.dma_start(out=g.X[k * 128:(k + 1) * 128, t0:t0 + NT], in_=xt[s][:, k, :]), bxt[s])
        P.end_stage()


def stage_p3b(g, gain_ap, win_ap, cw_ap, cbias_ap, wff_ap):
    nc, P, S = g.nc, g.P, g.S
    KC, FC, NT = 8, D_FF // 128, 256
    ps, bps = g.ps, g.bps
    with ExitStack() as st:
        Wi = _sb(nc, st, "p3Wi", [128, KC, 2 * D_FF], BF16)
        Wf = _sb(nc, st, "p3Wf", [128, FC, D_MODEL], BF16)
        gs = _sb(nc, st, "p3gs", [128, KC], F32)
        cw = _sb(nc, st, "p3cw", [128, 3, FC], F32)
        cbs = _sb(nc, st, "p3cb", [128, FC], F32)
        bWi = [P.buf("Wi%d" % k) for k in range(KC)]
        bWf = [P.buf("Wf%d" % k) for k in range(FC)]
        bsm = P.buf("small")
        P.load("sync", lambda e: e.dma_start(out=gs[:], in_=gain_ap.rearrange("(k p) -> p k", p=128), allow_slow_non_contiguous=True), bsm)
        P.load("sync", lambda e: e.dma_start(out=cw[:], in_=cw_ap.rearrange("j (c p) -> p j c", p=128), allow_slow_non_contiguous=True), bsm)
        P.load("sync", lambda e: e.dma_start(out=cbs[:], in_=cbias_ap.rearrange("(c p) -> p c", p=128), allow_slow_non_contiguous=True), bsm)
        with ExitStack() as st2:
            _load_cast_rows(P, nc, st2, "p3bi", Wi, bWi, win_ap, KC, 2 * D_FF, 1408, scale_ap=gs, scale_buf=bsm)
            _load_cast_rows(P, nc, st2, "p3bf", Wf, bWf, wff_ap, FC, D_MODEL, D_MODEL)
            P.barrier()
            P.emit()
        xt = [_sb(nc, st, "p3bxt%d" % i, [128, KC, NT], F32) for i in range(2)]
        sq = _sb(nc, st, "p3bsq", [128, KC, NT], BF16)
        h2 = _sb(nc, st, "p3bh2", [128, KC, NT], BF16)
        rb = _sb(nc, st, "p3brb", [128, NT], F32)
        act = _sb(nc, st, "p3bact", [128, FC, NT], BF16)
        gb = [_sb(nc, st, "p3bgb%d" % i, [128, NT + 2], F32) for i in range(2)]
        acc = [_sb(nc, st, "p3bacc%d" % i, [128, NT], F32) for i in range(2)]
        carry = _sb(nc, st, "p3bcar", [128, FC, 2], F32)
        bxt = [P.buf("xt%d" % i) for i in range(2)]
        bsq, bh2, brb = P.buf("sq"), P.buf("h2"), P.buf("rb")
        bact = [P.buf("act%d" % c) for c in range(FC)]
        bgb = [P.buf("gb%d" % i) for i in range(2)]
        bacc = [P.buf("acc%d" % i) for i in range(2)]
        bcar = [P.buf("car%d" % c) for c in range(FC)]
        for c in range(FC):
            P.op("vector", lambda e, c=c: e.memset(carry[:, c, :], 0.0), writes=[bcar[c]])
        xv = g.X.rearrange("(k p) t -> p k t", p=128)
        pi = 0
        gi = 0
        for t in range(S // NT):
            s = t % 2
            t0 = t * NT
            for k in range(KC):
                P.load("sync", lambda e, s=s, t0=t0, k=k: e.dma_start(out=xt[s][:, k, :], in_=g.X[k * 128:(k + 1) * 128, t0:t0 + NT]), bxt[s])
            P.op("scalar", lambda e, s=s: e.activation(out=sq[:], in_=xt[s][:], func=AF.Square), reads=[bxt[s]], writes=[bsq])
            for k in range(KC):
                P.op("tensor", lambda e, k=k: e.matmul(ps[0][:, 0:NT], g.cb[:, 3, :], sq[:, k, :], start=(k == 0), stop=(k == KC - 1)),
                     reads=[bsq, g.bcb], writes=[bps[0]], inc=(k == KC - 1))
            P.op("scalar", lambda e: e.activation(out=rb[:], in_=ps[0][:, 0:NT], func=AF.Sqrt, scale=1.0 / D_MODEL, bias=NORM_EPS),
                 reads=[bps[0]], writes=[brb])
            P.op("vector", lambda e: e.reciprocal(rb[:], rb[:]), reads=[brb], writes=[brb])
            for k in range(KC):
                P.op("vector", lambda e, k=k, s=s: e.tensor_tensor(h2[:, k, :], xt[s][:, k, :], rb[:], ALU.mult),
                     reads=[bxt[s], brb], writes=[bh2])
            for c in range(FC):
                pu = 1 + pi % 7
                pi += 1
                pg = 1 + pi % 7
                pi += 1
                q = gi % 2
                gi += 1
                for k in range(KC):
                    P.op("tensor", lambda e, k=k, c=c, pu=pu: e.matmul(ps[pu][:, 0:NT], Wi[:, k, c * 128:(c + 1) * 128], h2[:, k, :], start=(k == 0), stop=(k == KC - 1)),
                         reads=[bh2, bWi[k]], writes=[bps[pu]], inc=(k == KC - 1))
                for k in range(KC):
                    P.op("tensor", lambda e, k=k, c=c, pg=pg: e.matmul(ps[pg][:, 0:NT], Wi[:, k, D_FF + c * 128:D_FF + (c + 1) * 128], h2[:, k, :], start=(k == 0), stop=(k == KC - 1)),
                         reads=[bh2, bWi[k]], writes=[bps[pg]], inc=(k == KC - 1))
                P.op("scalar", lambda e, q=q, pg=pg: e.activation(out=gb[q][:, 2:NT + 2], in_=ps[pg][:, 0:NT], func=AF.Copy), reads=[bps[pg]], writes=[bgb[q]])
                P.op("vector", lambda e, q=q, c=c: e.tensor_copy(gb[q][:, 0:2], carry[:, c, :]), reads=[bcar[c]], writes=[bgb[q]])
                P.op("vector", lambda e, q=q, c=c: e.tensor_scalar(acc[q][:], gb[q][:, 2:NT + 2], cw[:, 2, c:c + 1], cbs[:, c:c + 1], ALU.mult, ALU.add),
                     reads=[bgb[q], bsm], writes=[bacc[q]])
                P.op("vector", lambda e, q=q, c=c: e.scalar_tensor_tensor(acc[q][:], gb[q][:, 1:NT + 1], cw[:, 1, c:c + 1], acc[q][:], ALU.mult, ALU.add),
                     reads=[bgb[q], bsm, bacc[q]], writes=[bacc[q]])
                P.op("vector", lambda e, q=q, c=c: e.scalar_tensor_tensor(acc[q][:], gb[q][:, 0:NT], cw[:, 0, c:c + 1], acc[q][:], ALU.mult, ALU.add),
                     reads=[bgb[q], bsm, bacc[q]], writes=[bacc[q]])
                P.op("vector", lambda e, q=q, c=c: e.tensor_copy(carry[:, c, :], gb[q][:, NT:NT + 2]), reads=[bgb[q]], writes=[bcar[c]])
                P.op("scalar", lambda e, q=q: e.activation(out=acc[q][:], in_=acc[q][:], func=AF.Silu), reads=[bacc[q]], writes=[bacc[q]])
                P.op("vector", lambda e, q=q, c=c, pu=pu: e.tensor_tensor(act[:, c, :], ps[pu][:, 0:NT], acc[q][:], ALU.mult),
                     reads=[bps[pu], bacc[q]], writes=[bact[c]])
            for m in range(KC):
                pb = 1 + pi % 7
                pi += 1
                for c in range(FC):
                    P.op("tensor", lambda e, c=c, m=m, pb=pb: e.matmul(ps[pb][:, 0:NT], Wf[:, c, m * 128:(m + 1) * 128], act[:, c, :], start=(c == 0), stop=(c == FC - 1)),
                         reads=[bact[c], bWf[c]], writes=[bps[pb]], inc=(c == FC - 1))
                P.op("vector", lambda e, m=m, s=s, pb=pb: e.tensor_tensor(xt[s][:, m, :], ps[pb][:, 0:NT], xt[s][:, m, :], ALU.add),
                     reads=[bps[pb], bxt[s]], writes=[bxt[s]])
            for k in range(KC):
                P.store("gpsimd" if k % 2 == 0 else "sync", lambda e, s=s, t0=t0, k=k: e.dma_start(out=g.X[k * 128:(k + 1) * 128, t0:t0 + NT], in_=xt[s][:, k, :]), bxt[s])
        P.end_stage()


def stage_final(g, gain_ap, out_ap):
    nc, P, S = g.nc, g.P, g.S
    KC, NT = 8, 512
    ps, bps = g.ps, g.bps
    with ExitStack() as st:
        gs = _sb(nc, st, "fngs", [128, KC], F32)
        bgs = P.buf("gs")
        P.load("sync", lambda e: e.dma_start(out=gs[:], in_=gain_ap.rearrange("(k p) -> p k", p=128), allow_slow_non_contiguous=True), bgs)
        xt = [_sb(nc, st, "fnxt%d" % i, [128, KC, NT], F32) for i in range(2)]
        sq = [_sb(nc, st, "fnsq%d" % i, [128, KC, NT], BF16) for i in range(2)]
        rb = [_sb(nc, st, "fnrb%d" % i, [128, NT], F32) for i in range(2)]
        bxt, bsq, brb = ([P.buf("f%d" % i) for i in range(2)] for _ in range(3))
        xv = g.X.rearrange("(k p) t -> p k t", p=128)
        ov = out_ap.rearrange("(k p) t -> p k t", p=128)
        for t in range(S // NT):
            s = t % 2
            t0 = t * NT
            pb = t % 2
            for k in range(KC):
                P.load("sync", lambda e, s=s, t0=t0, k=k: e.dma_start(out=xt[s][:, k, :], in_=g.X[k * 128:(k + 1) * 128, t0:t0 + NT]), bxt[s])
            P.op("scalar", lambda e, s=s: e.activation(out=sq[s][:], in_=xt[s][:], func=AF.Square), reads=[bxt[s]], writes=[bsq[s]])
            for k in range(KC):
                P.op("tensor", lambda e, k=k, s=s, pb=pb: e.matmul(ps[pb][:, 0:NT], g.cb[:, 3, :], sq[s][:, k, :], start=(k == 0), stop=(k == KC - 1)),
                     reads=[bsq[s], g.bcb], writes=[bps[pb]], inc=(k == KC - 1))
            P.op("scalar", lambda e, s=s, pb=pb: e.activation(out=rb[s][:], in_=ps[pb][:, 0:NT], func=AF.Sqrt, scale=1.0 / D_MODEL, bias=NORM_EPS),
                 reads=[bps[pb]], writes=[brb[s]])
            P.op("vector", lambda e, s=s: e.reciprocal(rb[s][:], rb[s][:]), reads=[brb[s]], writes=[brb[s]])
            for k in range(KC):
                P.op("vector", lambda e, k=k, s=s: e.scalar_tensor_tensor(xt[s][:, k, :], xt[s][:, k, :], gs[:, k:k + 1], rb[s][:], ALU.mult, ALU.mult),
                     reads=[bxt[s], brb[s], bgs], writes=[bxt[s]])
            for k in range(KC):
                P.store("gpsimd" if k % 2 == 0 else "sync", lambda e, s=s, t0=t0, k=k: e.dma_start(out=out_ap[k * 128:(k + 1) * 128, t0:t0 + NT], in_=xt[s][:, k, :]), bxt[s])
        P.end_stage()


def stage_moba(g, gain_ap, c_ohm):
    nc, P, S, NQ = g.nc, g.P, g.S, g.NQ
    NB = S // 256
    ps, bps = g.ps, g.bps
    ptb = g.ps[7][:].bitcast(BF16)
    with ExitStack() as st:
        KX = _sb(nc, st, "mbKX", [128, S], BF16)
        QX = _sb(nc, st, "mbQX", [128, S], BF16)
        Vp = _sb(nc, st, "mbVp", [128, NQ, 65], BF16)
        kmf = _sb(nc, st, "mbkmf", [64, NB], F32)
        kmb = _sb(nc, st, "mbkmb", [64, NB], BF16)
        G32 = [_sb(nc, st, "mbG32%d" % i, [128, 32], F32) for i in range(2)]
        m8 = [_sb(nc, st, "mbm8%d" % i, [128, 8], F32) for i in range(2)]
        NMW = [_sb(nc, st, "mbNMW%d" % i, [128, 96], BF16) for i in range(2)]
        PT = [_sb(nc, st, "mbPT%d" % i, [128, 512], BF16) for i in range(3)]
        gainb = _sb(nc, st, "mbgain", [128, 384], F32)
        fs = [_sb(nc, st, "mbfs%d" % i, [128, 4], F32) for i in range(2)]
        o32 = [_sb(nc, st, "mbo32%d" % i, [128, 64], F32) for i in range(2)]
        junk = _sb(nc, st, "mbjunk", [128, 64], F32)
        ob = [_sb(nc, st, "mbob%d" % i, [128, 64], BF16) for i in range(2)]
        OT = [_sb(nc, st, "mbOT%d" % i, [64, 512], BF16) for i in range(2)]
        B = P.buf
        bKXk, bKXo, bQXq, bVp, bVp1 = B("KXk"), B("KXo"), B("QXq"), B("Vp"), B("Vp1")
        bQXm = [B("QXm%d" % i) for i in range(NQ)]
        bkm, bgain = B("km"), B("gain")
        bG32 = [B("G32%d" % i) for i in range(2)]
        bm8 = [B("m8%d" % i) for i in range(2)]
        bNMW = [B("NMW%d" % i) for i in range(2)]
        bPT = [B("PT%d" % i) for i in range(3)]
        bfs = [B("fs%d" % i) for i in range(2)]
        bo32 = [B("o32%d" % i) for i in range(2)]
        bjunk = B("junk")
        bob = [B("ob%d" % i) for i in range(2)]
        bOT = [B("OT%d" % i) for i in range(2)]

        P.load("sync", lambda e: e.dma_start(out=KX[64:96, :], in_=c_ohm[:, :]), bKXo)
        P.load("sync", lambda e: e.dma_start(out=gainb[:], in_=gain_ap.partition_broadcast(128), allow_slow_non_contiguous=True), bgain)
        P.op("vector", lambda e: e.memset(QX[64:96, :], 0.0), writes=bQXm)
        P.op("vector", lambda e: e.memset(Vp[:, :, 64:65], 1.0), writes=[bVp1])
        tv = g.TMV.rearrange("(t p) d -> p t d", p=128)
        pti = 0
        sbank = 0
        for h in range(6):
            P.load("sync", lambda e, h=h: e.dma_start(out=KX[0:64, :], in_=g.FM[10 * 128 + h * 64:10 * 128 + (h + 1) * 64, :]), bKXk)
            P.load("gpsimd", lambda e, h=h: e.dma_start(out=QX[0:64, :], in_=g.FM[7 * 128 + h * 64:7 * 128 + (h + 1) * 64, :]), bQXq)
            for t_ in range(0, NQ, 16):
                P.load("sync", lambda e, h=h, t_=t_: e.dma_start(out=Vp[:, t_:t_ + 16, 0:64], in_=tv[:, t_:t_ + 16, 256 + h * 64:256 + (h + 1) * 64]), bVp)
            P.op("vector", lambda e: e.tensor_reduce(kmf[:, :], KX[0:64, :].rearrange("p (n k) -> p n k", k=256), AX.X, ALU.add),
                 reads=[bKXk], writes=[bkm])
            P.op("vector", lambda e: e.tensor_scalar(kmb[:, :], kmf[:, :], 1.0 / 256, None, ALU.mult), reads=[bkm], writes=[bkm])
            for v in range(2):
                P.op("vector", lambda e, v=v: e.memset(G32[v][:], -1e30), writes=[bG32[v]])
                P.op("vector", lambda e, v=v: e.memset(NMW[v][:], 0.0), writes=[bNMW[v]])
            def chain_front(qt):
                cur = qt // 2
                if qt >= NQ or cur == 0:
                    return
                qs = slice(qt * 128, (qt + 1) * 128)
                v = qt % 2
                P.op("tensor", lambda e, qs=qs: e.matmul(ps[6][:, 0:NB], QX[0:64, qs], kmb[0:64, 0:NB], start=True, stop=True),
                     reads=[bQXq, bkm], writes=[bps[6]])
                P.op("vector", lambda e, cur=cur, v=v: e.tensor_copy(G32[v][:, 0:cur], ps[6][:, 0:cur]), reads=[bps[6]], writes=[bG32[v]])
                P.op("vector", lambda e, v=v: e.max(out=m8[v][:], in_=G32[v][:, 0:32]), reads=[bG32[v]], writes=[bm8[v]])
                P.op("vector", lambda e, cur=cur, v=v: e.tensor_scalar(NMW[v][:, 64:64 + cur], G32[v][:, 0:cur], m8[v][:, 2:3], MNEG, ALU.is_lt, ALU.mult),
                     reads=[bG32[v], bm8[v]], writes=[bNMW[v]])

            def chain_back(qt):
                cur = qt // 2
                if qt >= NQ or cur == 0:
                    return
                qs = slice(qt * 128, (qt + 1) * 128)
                v = qt % 2
                P.op("tensor", lambda e, v=v: e.transpose(ptb[0:96, 0:128], NMW[v][:, 0:96], g.cb[:, 0, :]), reads=[bNMW[v], g.bcb], writes=[bps[7]])
                P.op("scalar", lambda e, qs=qs: e.activation(out=QX[64:96, qs], in_=ptb[64:96, 0:128], func=AF.Copy), reads=[bps[7]], writes=[bQXm[qt]])

            for q_ in range(3):
                chain_front(q_)
            for q_ in range(2):
                chain_back(q_)
            for qt in range(NQ):
                chain_front(qt + 3)
                chain_back(qt + 2)
                qs = slice(qt * 128, (qt + 1) * 128)
                ob_ = 4 + qt % 2
                f = qt % 2
                for g0 in range(0, qt + 1, 4):
                    kts = list(range(g0, min(g0 + 4, qt + 1)))
                    bank = sbank % 4
                    sbank += 1
                    mms = []
                    for j, kt in enumerate(kts):
                        diag = (kt == qt)
                        mms.append((lambda e, j=j, kt=kt, qs=qs, bank=bank, diag=diag: e.matmul(
                            ps[bank][:, j * 128:(j + 1) * 128], KX[0:96, kt * 128:(kt + 1) * 128], QX[0:96, qs],
                            start=(j == 0), stop=(not diag), skip_group_check=True), [bKXk, bKXo, bQXq, bQXm[qt]]))
                        if diag:
                            mms.append((lambda e, j=j, bank=bank: e.matmul(
                                ps[bank][:, j * 128:(j + 1) * 128], g.cb[:, 0, :], g.cb[:, 1, :], start=False, stop=True, skip_group_check=True),
                                [g.bcb]))
                    for i_, (fn_, rd_) in enumerate(mms):
                        P.op("tensor", fn_, reads=rd_, writes=[bps[bank]], inc=(i_ == len(mms) - 1))
                    n = len(kts)
                    pt = pti % 3
                    pti += 1
                    P.op("scalar", lambda e, bank=bank, n=n, pt=pt: e.activation(out=PT[pt][:, 0:n * 128], in_=ps[bank][:, 0:n * 128], func=AF.Exp, scale=0.125),
                         reads=[bps[bank]], writes=[bPT[pt]])
                    for j, kt in enumerate(kts):
                        P.op("tensor", lambda e, j=j, kt=kt, pt=pt, ob_=ob_, qt=qt: e.matmul(
                            ps[ob_][:, 0:65], PT[pt][:, j * 128:(j + 1) * 128], Vp[:, kt, :], start=(kt == 0), stop=(kt == qt)),
                            reads=[bPT[pt], bVp, bVp1], writes=[bps[ob_]], inc=(kt == qt or j == len(kts) - 1))
                P.op("vector", lambda e, f=f, ob_=ob_: e.tensor_scalar(fs[f][:, 0:1], ps[ob_][:, 64:65], 1e-30, None, ALU.max), reads=[bps[ob_]], writes=[bfs[f]])
                P.op("vector", lambda e, f=f: e.reciprocal(fs[f][:, 0:1], fs[f][:, 0:1]), reads=[bfs[f]], writes=[bfs[f]])
                P.op("vector", lambda e, f=f, ob_=ob_: e.tensor_scalar(o32[f][:], ps[ob_][:, 0:64], fs[f][:, 0:1], None, ALU.mult),
                     reads=[bps[ob_], bfs[f]], writes=[bo32[f]])
                P.op("scalar", lambda e, f=f: e.activation(out=junk[:], in_=o32[f][:], func=AF.Square, accum_out=fs[f][:, 1:2]),
                     reads=[bo32[f]], writes=[bjunk, bfs[f]])
                P.op("scalar", lambda e, f=f: e.activation(out=fs[f][:, 2:3], in_=fs[f][:, 1:2], func=AF.Sqrt, scale=1.0 / 64, bias=NORM_EPS),
                     reads=[bfs[f]], writes=[bfs[f]])
                P.op("vector", lambda e, f=f: e.reciprocal(fs[f][:, 3:4], fs[f][:, 2:3]), reads=[bfs[f]], writes=[bfs[f]])
                P.op("vector", lambda e, f=f, h=h: e.scalar_tensor_tensor(ob[f][:], o32[f][:], fs[f][:, 3:4], gainb[:, h * 64:(h + 1) * 64], ALU.mult, ALU.mult),
                     reads=[bo32[f], bfs[f], bgain], writes=[bob[f]])
                q4 = qt % 4
                P.op("tensor", lambda e, f=f, q4=q4: e.transpose(ptb[0:64, 256 + q4 * 128:256 + (q4 + 1) * 128], ob[f][:], g.cb[:, 0, :]),
                     reads=[bob[f], g.bcb], writes=[bps[7]])
                if q4 == 3:
                    oo = (qt // 4) % 2
                    P.op("scalar", lambda e, oo=oo: e.activation(out=OT[oo][:], in_=ptb[0:64, 256:768], func=AF.Copy), reads=[bps[7]], writes=[bOT[oo]])
                    c0 = (qt - 3) * 128
                    P.store("gpsimd", lambda e, oo=oo, h=h, c0=c0: e.dma_start(out=g.OMT[640 + h * 64:640 + (h + 1) * 64, c0:c0 + 512], in_=OT[oo][:]), bOT[oo])
        P.end_stage()


def host_consts_nsa(S):
    import ml_dtypes
    key = np.arange(S)
    ohs = ((key[None, :] // 64) % 64 == np.arange(64)[:, None]).astype(np.float32)
    n = np.arange(128)
    c = np.arange(2176)
    cm = np.where(16 * n[:, None] + 31 <= c[None, :], 0.0, MNEG).astype(np.float32)
    n_cmp = S // 16 - 1
    n_slc = S // 64
    NCT = (n_cmp + 1 + 127) // 128
    r_, c_ = 4, 2
    ii = (r_ * np.arange(n_slc)[:, None, None] - np.arange(r_)[None, :, None] - np.arange(c_)[None, None, :]).reshape(n_slc, -1)
    mm = (ii[:, :, None] == np.arange(n_cmp)[None, None, :]).sum(1).T.astype(np.float32)
    cmap = np.zeros((NCT * 128, 128), np.float32)
    cmap[:n_cmp, :n_slc] = mm
    cmap = cmap.reshape(NCT, 128, 128).transpose(1, 0, 2)
    BIG = 1e9
    fbw = np.zeros((128, 256), np.float32)
    cc = np.arange(256)
    for i in range(128):
        cur = 128 if i < 64 else 129
        fbw[i] = np.where(cc > cur, -BIG, np.where(cc >= cur - 1, BIG, 0.0))
    return {"c_ohs": ohs.astype(ml_dtypes.bfloat16), "c_cm": cm.astype(ml_dtypes.bfloat16),
            "c_cmap": np.ascontiguousarray(cmap).astype(ml_dtypes.bfloat16), "c_fbw": fbw}


def stage_nsa(g, gain_ap, pos_ap, w1_ap, w2_ap, c_ohs, c_cm, c_cmap, c_fbw):
    nc, P, S, NQ = g.nc, g.P, g.S, g.NQ
    NC = S // 16 - 1
    NCT = (NC + 1 + 127) // 128
    ps, bps = g.ps, g.bps
    ptb = g.ps[7][:].bitcast(BF16)
    B = P.buf
    with ExitStack() as st:
        KSX = _sb(nc, st, "nsKSX", [128, S], BF16)
        KW = _sb(nc, st, "nsKW", [64, S], BF16)
        KVT = _sb(nc, st, "nsKVT", [64, S], BF16)
        VSp = _sb(nc, st, "nsVSp", [128, NQ, 65], BF16)
        VWp = _sb(nc, st, "nsVWp", [128, NQ, 65], BF16)
        KC = _sb(nc, st, "nsKC", [64, NCT * 128], BF16)
        Vcp = _sb(nc, st, "nsVcp", [128, NCT, 65], BF16)
        CM = _sb(nc, st, "nsCM", [128, 2176], BF16)
        CMAP = _sb(nc, st, "nsCMAP", [128, NCT, 128], BF16)
        FBW = _sb(nc, st, "nsFBW", [128, 256], F32)
        TRI = _sb(nc, st, "nsTRI", [128, 2, 384], BF16)
        gainb = _sb(nc, st, "nsgain", [128, 384], F32)
        W1s = _sb(nc, st, "nsW1s", [64, 32, 256], F32)
        W1b = _sb(nc, st, "nsW1b", [64, 32, 256], BF16)
        W2s = _sb(nc, st, "nsW2s", [128, 2, 64], F32)
        W2b = _sb(nc, st, "nsW2b", [128, 2, 64], BF16)
        pss = _sb(nc, st, "nspss", [64, 32], F32)
        psb = _sb(nc, st, "nspsb", [64, 32], BF16)
        bsb = _sb(nc, st, "nsbsb", [128, 2], F32)
        xb = _sb(nc, st, "nsxb", [128, 512], F32)
        x2 = _sb(nc, st, "nsx2", [128, 512], F32)
        ha = _sb(nc, st, "nsha", [128, 2, 512], BF16)
        QXA = [_sb(nc, st, "nsQXA%d" % i, [128, 384], BF16) for i in range(2)]
        QXB = [_sb(nc, st, "nsQXB%d" % i, [128, 384], BF16) for i in range(2)]
        PT = [_sb(nc, st, "nsPT%d" % i, [128, 384], BF16) for i in range(3)]
        gts = [_sb(nc, st, "nsgt%d" % i, [128, 18], F32) for i in range(2)]
        L9 = [_sb(nc, st, "nsL9%d" % i, [128, 9], F32) for i in range(2)]
        C9 = [_sb(nc, st, "nsC9%d" % i, [128, 9], F32) for i in range(2)]
        rlc = _sb(nc, st, "nsrlc", [128, 3], F32)
        imp = _sb(nc, st, "nsimp", [128, 128], F32)
        imp3 = _sb(nc, st, "nsimp3", [128, 128], F32)
        m8a = _sb(nc, st, "nsm8a", [128, 8], F32)
        m8b = _sb(nc, st, "nsm8b", [128, 8], F32)
        W192 = _sb(nc, st, "nsW192", [128, 192], BF16)
        WB = _sb(nc, st, "nsWB", [128, 128], BF16)
        bWB = B("WB")
        fs = [_sb(nc, st, "nsfs%d" % i, [128, 4], F32) for i in range(2)]
        o32 = [_sb(nc, st, "nso32%d" % i, [128, 64], F32) for i in range(2)]
        junk = _sb(nc, st, "nsjunk", [128, 64], F32)
        ob = [_sb(nc, st, "nsob%d" % i, [128, 64], BF16) for i in range(2)]
        OT = [_sb(nc, st, "nsOT%d" % i, [64, 3, 128], BF16) for i in range(2)]
        bKSk, bKSo, bKW, bKVT, bVS, bVW, bV1 = B("KSk"), B("KSo"), B("KW"), B("KVT"), B("VS"), B("VW"), B("V1")
        bKC, bVcp, bconst, bW1s, bW1b, bW2, bpos, bbsb, bxb, bx2, bha = (B("x") for _ in range(11))
        bQXAq = [B("QXAq%d" % i) for i in range(2)]
        bQXAm = [B("QXAm%d" % i) for i in range(2)]
        bQXBq = [B("QXBq%d" % i) for i in range(2)]
        bQXBm = [B("QXBm%d" % i) for i in range(2)]
        bPT = [B("PT%d" % i) for i in range(3)]
        bgts = [B("gt%d" % i) for i in range(2)]
        bL9 = [B("L9%d" % i) for i in range(2)]
        bC9 = [B("C9%d" % i) for i in range(2)]
        brlc, bimp, bimp3, bm8a, bm8b, bW192, bjunk = (B("y") for _ in range(7))
        bfs = [B("fs%d" % i) for i in range(2)]
        bo32 = [B("o32%d" % i) for i in range(2)]
        bob = [B("ob%d" % i) for i in range(2)]
        bOT = [B("OT%d" % i) for i in range(2)]

        P.load("sync", lambda e: e.dma_start(out=KSX[64:128, :], in_=c_ohs[:, :]), bKSo)
        P.load("sync", lambda e: e.dma_start(out=CM[:], in_=c_cm[:, :]), bconst)
        P.load("sync", lambda e: e.dma_start(out=CMAP[:], in_=c_cmap[:, :, :]), bconst)
        P.load("sync", lambda e: e.dma_start(out=FBW[:], in_=c_fbw[:, :]), bconst)
        P.load("sync", lambda e: e.dma_start(out=gainb[:], in_=gain_ap.partition_broadcast(128), allow_slow_non_contiguous=True), bconst)
        for r in range(3):
            P.op("vector", lambda e, r=r: e.tensor_copy(TRI[:, 0, r * 128:(r + 1) * 128], g.cb[:, 1, :]), reads=[g.bcb], writes=[bconst])
            P.op("vector", lambda e, r=r: e.tensor_copy(TRI[:, 1, r * 128:(r + 1) * 128], g.cb[:, 2, :]), reads=[g.bcb], writes=[bconst])
        P.op("vector", lambda e: e.memset(VSp[:, :, 64:65], 1.0), writes=[bV1])
        P.op("vector", lambda e: e.memset(VWp[:, :, 64:65], 1.0), writes=[bV1])
        P.op("vector", lambda e: e.memset(Vcp[:, :, 64:65], 1.0), writes=[bV1])
        P.op("vector", lambda e: e.memset(ha[:], 0.0), writes=[bha])
        P.op("vector", lambda e: e.memset(W192[:], 0.0), writes=[bW192])
        P.op("vector", lambda e: e.memset(WB[:], 0.0), writes=[bWB])
        tv = g.TMV.rearrange("(t p) d -> p t d", p=128)
        sbank = 0
        pti = 0

        def attn_tile(lhs_fn, lhs_reads, K, rhs_t, rhs_reads, Vt, vreads, kt, obank, ocol, first_o, last_o, masks):
            nonlocal sbank, pti
            bank = sbank % 2
            sbank += 1
            mms = [(lambda e, bank=bank: e.matmul(ps[bank][:, 0:384], lhs_fn(), rhs_t[0:K, 0:384], start=True, stop=(len(masks) == 0), skip_group_check=True),
                    list(lhs_reads) + list(rhs_reads))]
            for (mfn, mreads) in masks:
                mms.append((lambda e, bank=bank, mfn=mfn: mfn(e, ps[bank]), mreads))
            for i_, (fn_, rd_) in enumerate(mms):
                P.op("tensor", fn_, reads=rd_, writes=[bps[bank]], inc=(i_ == len(mms) - 1))
            pt = pti % 3
            pti += 1
            P.op("scalar", lambda e, bank=bank, pt=pt: e.activation(out=PT[pt][:], in_=ps[bank][:, 0:384], func=AF.Exp, scale=0.125),
                 reads=[bps[bank]], writes=[bPT[pt]])
            for r in range(3):
                P.op("tensor", lambda e, r=r, pt=pt: e.matmul(ps[obank][:, ocol + r * 65:ocol + (r + 1) * 65], PT[pt][:, r * 128:(r + 1) * 128], Vt,
                                                             start=(first_o and r == 0), stop=last_o, skip_group_check=True),
                     reads=[bPT[pt]] + list(vreads), writes=[bps[obank]], inc=(r == 2))
            return pt

        for g_ in range(2):
            P.load("sync", lambda e, g_=g_: e.dma_start(out=KSX[0:64, :], in_=g.FM[5 * 128 + g_ * 64:5 * 128 + (g_ + 1) * 64, :]), bKSk)
            P.load("gpsimd", lambda e, g_=g_: e.dma_start(out=KW[:, :], in_=g.FM[6 * 128 + g_ * 64:6 * 128 + (g_ + 1) * 64, :]), bKW)
            for t_ in range(0, NQ, 16):
                P.load("sync", lambda e, g_=g_, t_=t_: e.dma_start(out=VSp[:, t_:t_ + 16, 0:64], in_=tv[:, t_:t_ + 16, g_ * 64:(g_ + 1) * 64]), bVS)
                P.load("gpsimd", lambda e, g_=g_, t_=t_: e.dma_start(out=VWp[:, t_:t_ + 16, 0:64], in_=tv[:, t_:t_ + 16, 128 + g_ * 64:128 + (g_ + 1) * 64]), bVW)
            for j in range(2):
                P.load("sync", lambda e, g_=g_, j=j: e.dma_start(out=KVT[:, :], in_=g.FM[(3 + j) * 128 + g_ * 64:(3 + j) * 128 + (g_ + 1) * 64, :]), bKVT)
                P.load("gpsimd", lambda e, j=j: e.dma_start(out=W1s[:], in_=w1_ap[j].rearrange("(l d) h -> d l h", d=64)), bW1s)
                P.load("sync", lambda e, j=j: e.dma_start(out=W2s[:], in_=w2_ap[j].rearrange("(c p) d -> p c d", p=128)), bW2)
                P.load("sync", lambda e, j=j: e.dma_start(out=pss[:], in_=pos_ap[j].rearrange("l d -> d l"), allow_slow_non_contiguous=True), bpos)
                P.op("vector", lambda e: e.tensor_copy(W1b[:], W1s[:]), reads=[bW1s], writes=[bW1b])
                P.op("vector", lambda e: e.tensor_copy(W2b[:], W2s[:]), reads=[bW2], writes=[bW2])
                P.op("vector", lambda e: e.tensor_copy(psb[:], pss[:]), reads=[bpos], writes=[bpos])
                for half in range(2):
                    for l in range(32):
                        P.op("tensor", lambda e, half=half, l=l: e.matmul(ps[2][:, half:half + 1], W1b[:, l, half * 128:(half + 1) * 128], psb[:, l:l + 1],
                                                                         start=(half == 0 and l == 0), stop=(l == 31), skip_group_check=True),
                             reads=[bW1b, bpos], writes=[bps[2]], inc=(half == 1 and l == 31))
                P.op("vector", lambda e: e.tensor_copy(bsb[:], ps[2][:, 0:2]), reads=[bps[2]], writes=[bbsb])
                for half in range(2):
                    hb = 5 + half
                    for l in range(32):
                        P.op("tensor", lambda e, half=half, l=l, hb=hb: e.matmul(ps[hb][:, 0:NC], W1b[:, l, half * 128:(half + 1) * 128], KVT[0:64, l:l + 16 * (NC - 1) + 1:16],
                                                                                start=(l == 0), stop=(l == 31)),
                             reads=[bW1b, bKVT], writes=[bps[hb]], inc=(l == 31))
                    P.op("scalar", lambda e, half=half, hb=hb: e.activation(out=xb[:, 0:NC], in_=ps[hb][:, 0:NC], func=AF.Identity, bias=bsb[:, half:half + 1]),
                         reads=[bps[hb], bbsb], writes=[bxb])
                    P.op("scalar", lambda e: e.activation(out=x2[:, 0:NC], in_=xb[:, 0:NC], func=AF.Square), reads=[bxb], writes=[bx2])
                    P.op("vector", lambda e: e.tensor_scalar(x2[:, 0:NC], x2[:, 0:NC], 0.044715, 1.0, ALU.mult, ALU.add), reads=[bx2], writes=[bx2])
                    P.op("vector", lambda e: e.tensor_tensor(x2[:, 0:NC], x2[:, 0:NC], xb[:, 0:NC], ALU.mult), reads=[bx2, bxb], writes=[bx2])
                    P.op("scalar", lambda e: e.activation(out=x2[:, 0:NC], in_=x2[:, 0:NC], func=AF.Sigmoid, scale=1.5957691216), reads=[bx2], writes=[bx2])
                    P.op("vector", lambda e, half=half: e.tensor_tensor(ha[:, half, 0:NC], x2[:, 0:NC], xb[:, 0:NC], ALU.mult), reads=[bx2, bxb], writes=[bha])
                if j == 0:
                    for half in range(2):
                        P.op("tensor", lambda e, half=half: e.matmul(ps[5][0:64, 0:NCT * 128], W2b[:, half, :], ha[:, half, 0:NCT * 128], start=(half == 0), stop=(half == 1)),
                             reads=[bW2, bha], writes=[bps[5]], inc=(half == 1))
                    P.op("vector", lambda e: e.tensor_copy(KC[:, :], ps[5][0:64, 0:NCT * 128]), reads=[bps[5]], writes=[bKC])
                else:
                    for nt in range(NCT):
                        for half in range(2):
                            P.op("tensor", lambda e, half=half, nt=nt: e.matmul(ps[6][:, 0:64], ha[:, half, nt * 128:(nt + 1) * 128], W2b[:, half, :], start=(half == 0), stop=(half == 1)),
                                 reads=[bW2, bha], writes=[bps[6]], inc=(half == 1))
                        P.op("vector", lambda e, nt=nt: e.tensor_copy(Vcp[:, nt, 0:64], ps[6][:, 0:64]), reads=[bps[6]], writes=[bVcp])
            for qt in range(NQ):
                a = qt % 2
                qs = slice(qt * 128, (qt + 1) * 128)
                needB = qt >= 32
                for r in range(3):
                    hh = g_ * 3 + r
                    P.load("sync", lambda e, a=a, r=r, hh=hh, qs=qs: e.dma_start(out=QXA[a][0:64, r * 128:(r + 1) * 128], in_=g.FM[hh * 64:(hh + 1) * 64, qs]), bQXAq[a])
                P.load("gpsimd", lambda e, a=a, qs=qs: e.dma_start(out=gts[a][:], in_=g.GT[qs, :]), bgts[a])
                P.op("scalar", lambda e, a=a: e.activation(out=gts[a][:], in_=gts[a][:], func=AF.Sigmoid), reads=[bgts[a]], writes=[bgts[a]])
                nts = [nt for nt in range(NCT) if qt - 16 * nt >= 0]
                for ii, nt in enumerate(nts):
                    dlt = qt - 16 * nt
                    masks = []
                    if dlt <= 16:
                        for r in range(3):
                            masks.append((lambda e, pb, r=r, dlt=dlt: e.matmul(pb[:, r * 128:(r + 1) * 128], g.cb[:, 0, :], CM[:, 128 * dlt:128 * dlt + 128],
                                                                             start=False, stop=(r == 2), skip_group_check=True), [g.bcb, bconst]))
                    pt = attn_tile(lambda nt=nt: KC[0:64, nt * 128:(nt + 1) * 128], [bKC], 64, QXA[a], [bQXAq[a]], Vcp[:, nt, :], [bVcp, bV1], nt,
                                   3, 0, ii == 0, ii == len(nts) - 1, masks)
                    for r in range(3):
                        P.op("tensor", lambda e, r=r, pt=pt, nt=nt, ii=ii: e.matmul(ps[2][:, r * 128:(r + 1) * 128], PT[pt][:, r * 128:(r + 1) * 128], CMAP[:, nt, :],
                                                                                 start=(ii == 0 and r == 0), stop=(ii == len(nts) - 1), skip_group_check=True),
                             reads=[bPT[pt], bconst], writes=[bps[2]], inc=(r == 2))
                P.op("vector", lambda e: e.tensor_scalar(rlc[:], ps[3][:, 0:195].rearrange("p (r c) -> p r c", c=65)[:, :, 64], 1e-30, None, ALU.max),
                     reads=[bps[3]], writes=[brlc])
                P.op("vector", lambda e: e.reciprocal(rlc[:], rlc[:]), reads=[brlc], writes=[brlc])
                P.op("vector", lambda e: e.tensor_scalar(imp[:], ps[2][:, 0:128], rlc[:, 0:1], None, ALU.mult), reads=[bps[2], brlc], writes=[bimp])
                P.op("vector", lambda e: e.scalar_tensor_tensor(imp[:], ps[2][:, 128:256], rlc[:, 1:2], imp[:], ALU.mult, ALU.add), reads=[bps[2], brlc, bimp], writes=[bimp])
                P.op("vector", lambda e: e.scalar_tensor_tensor(imp[:], ps[2][:, 256:384], rlc[:, 2:3], imp[:], ALU.mult, ALU.add), reads=[bps[2], brlc, bimp], writes=[bimp])
                P.op("vector", lambda e, qt=qt: e.tensor_tensor(imp[:], imp[:], FBW[:, 128 - 2 * qt:256 - 2 * qt], ALU.add), reads=[bimp, bconst], writes=[bimp])
                P.op("vector", lambda e: e.memset(imp[:, 0:1], 1e9), writes=[bimp])
                P.op("vector", lambda e: e.max(out=m8a[:], in_=imp[:]), reads=[bimp], writes=[bm8a])
                P.op("vector", lambda e: e.match_replace(out=imp3[:], in_to_replace=m8a[:], in_values=imp[:], imm_value=-3e9), reads=[bimp, bm8a], writes=[bimp3])
                P.op("vector", lambda e: e.max(out=m8b[:], in_=imp3[:]), reads=[bimp3], writes=[bm8b])
                P.op("vector", lambda e: e.tensor_scalar(W192[:, 64:192], imp[:], m8b[:, 7:8], MNEG, ALU.is_lt, ALU.mult), reads=[bimp, bm8b], writes=[bW192])
                P.op("tensor", lambda e: e.transpose(ptb[:, 0:128], W192[:, 0:128], g.cb[:, 0, :]), reads=[bW192, g.bcb], writes=[bps[7]])
                if needB:
                    P.op("vector", lambda e: e.tensor_scalar(WB[:, 64:128], imp[:, 64:128], m8b[:, 7:8], MNEG, ALU.is_lt, ALU.mult), reads=[bimp, bm8b], writes=[bWB])
                for r in range(3):
                    P.op("scalar" if r != 1 else "vector",
                         (lambda e, a=a, r=r: e.activation(out=QXA[a][64:128, r * 128:(r + 1) * 128], in_=ptb[64:128, 0:128], func=AF.Copy)) if r != 1 else
                         (lambda e, a=a, r=r: e.tensor_copy(QXA[a][64:128, r * 128:(r + 1) * 128], ptb[64:128, 0:128])),
                         reads=[bps[7]], writes=[bQXAm[a]])
                wk = list(range(max(0, qt - 4), qt + 1))
                for ii, kt in enumerate(wk):
                    masks = []
                    if kt == qt:
                        masks.append((lambda e, pb: e.matmul(pb[:, 0:384], g.cb[:, 0, :], TRI[:, 0, :], start=False, stop=True, skip_group_check=True), [g.bcb, bconst]))
                    if kt == qt - 4:
                        masks.append((lambda e, pb: e.matmul(pb[:, 0:384], g.cb[:, 0, :], TRI[:, 1, :], start=False, stop=True, skip_group_check=True), [g.bcb, bconst]))
                    attn_tile(lambda kt=kt: KW[0:64, kt * 128:(kt + 1) * 128], [bKW], 64, QXA[a], [bQXAq[a]], VWp[:, kt, :], [bVW, bV1], kt,
                              3, 195, False, ii == len(wk) - 1, masks)
                for kt in range(qt + 1):
                    masks = []
                    if kt == qt:
                        masks.append((lambda e, pb: e.matmul(pb[:, 0:384], g.cb[:, 0, :], TRI[:, 0, :], start=False, stop=True, skip_group_check=True), [g.bcb, bconst]))
                    if kt == 32:
                        P.op("tensor", lambda e: e.transpose(ptb[:, 0:128], WB[:, 0:128], g.cb[:, 0, :]), reads=[bWB, g.bcb], writes=[bps[7]])
                        for r in range(3):
                            P.op("scalar" if r != 1 else "vector",
                                 (lambda e, a=a, r=r: e.activation(out=QXA[a][64:128, r * 128:(r + 1) * 128], in_=ptb[64:128, 0:128], func=AF.Copy)) if r != 1 else
                                 (lambda e, a=a, r=r: e.tensor_copy(QXA[a][64:128, r * 128:(r + 1) * 128], ptb[64:128, 0:128])),
                                 reads=[bps[7]], writes=[bQXAm[a]])
                    attn_tile(lambda kt=kt: KSX[:, kt * 128:(kt + 1) * 128], [bKSk, bKSo], 128, QXA[a],
                              [bQXAq[a], bQXAm[a]], VSp[:, kt, :], [bVS, bV1], kt,
                              4, 0, kt == 0, kt == qt, masks)
                pv3 = lambda bank, c0: ps[bank][:, c0:c0 + 195].rearrange("p (r c) -> p r c", c=65)
                P.op("vector", lambda e, a=a: e.tensor_scalar(L9[a][:, 0:3], pv3(3, 0)[:, :, 64], 1e-30, None, ALU.max), reads=[bps[3]], writes=[bL9[a]])
                P.op("vector", lambda e, a=a: e.tensor_scalar(L9[a][:, 3:6], pv3(4, 0)[:, :, 64], 1e-30, None, ALU.max), reads=[bps[4]], writes=[bL9[a]])
                P.op("vector", lambda e, a=a: e.tensor_scalar(L9[a][:, 6:9], pv3(3, 195)[:, :, 64], 1e-30, None, ALU.max), reads=[bps[3]], writes=[bL9[a]])
                P.op("vector", lambda e, a=a: e.reciprocal(L9[a][:], L9[a][:]), reads=[bL9[a]], writes=[bL9[a]])
                P.op("vector", lambda e, a=a, g_=g_: e.tensor_tensor(C9[a][:].rearrange("p (b r) -> p b r", r=3), L9[a][:].rearrange("p (b r) -> p b r", r=3),
                                                                  gts[a][:, g_ * 9:(g_ + 1) * 9].rearrange("p (r b) -> p b r", b=3), ALU.mult),
                     reads=[bL9[a], bgts[a]], writes=[bC9[a]])
                for r in range(3):
                    f = r % 2
                    hh = g_ * 3 + r
                    P.op("vector", lambda e, f=f, r=r, a=a: e.tensor_scalar(o32[f][:], ps[3][:, r * 65:r * 65 + 64], C9[a][:, r:r + 1], None, ALU.mult),
                         reads=[bps[3], bC9[a]], writes=[bo32[f]])
                    P.op("vector", lambda e, f=f, r=r, a=a: e.scalar_tensor_tensor(o32[f][:], ps[4][:, r * 65:r * 65 + 64], C9[a][:, 3 + r:4 + r], o32[f][:], ALU.mult, ALU.add),
                         reads=[bps[4], bC9[a], bo32[f]], writes=[bo32[f]])
                    P.op("vector", lambda e, f=f, r=r, a=a: e.scalar_tensor_tensor(o32[f][:], ps[3][:, 195 + r * 65:195 + r * 65 + 64], C9[a][:, 6 + r:7 + r], o32[f][:], ALU.mult, ALU.add),
                         reads=[bps[3], bC9[a], bo32[f]], writes=[bo32[f]])
                    P.op("scalar", lambda e, f=f: e.activation(out=junk[:], in_=o32[f][:], func=AF.Square, accum_out=fs[f][:, 1:2]),
                         reads=[bo32[f]], writes=[bjunk, bfs[f]])
                    P.op("scalar", lambda e, f=f: e.activation(out=fs[f][:, 2:3], in_=fs[f][:, 1:2], func=AF.Sqrt, scale=1.0 / 64, bias=NORM_EPS),
                         reads=[bfs[f]], writes=[bfs[f]])
                    P.op("vector", lambda e, f=f: e.reciprocal(fs[f][:, 3:4], fs[f][:, 2:3]), reads=[bfs[f]], writes=[bfs[f]])
                    P.op("vector", lambda e, f=f, hh=hh: e.scalar_tensor_tensor(ob[f][:], o32[f][:], fs[f][:, 3:4], gainb[:, hh * 64:(hh + 1) * 64], ALU.mult, ALU.mult),
                         reads=[bo32[f], bfs[f], bconst], writes=[bob[f]])
                    P.op("tensor", lambda e, f=f, r=r: e.transpose(ptb[0:64, 256 + r * 128:256 + (r + 1) * 128], ob[f][:], g.cb[:, 0, :]),
                         reads=[bob[f], g.bcb], writes=[bps[7]])
                P.op("scalar", lambda e, a=a: e.activation(out=OT[a][:].rearrange("p r t -> p (r t)"), in_=ptb[0:64, 256:640], func=AF.Copy), reads=[bps[7]], writes=[bOT[a]])
                for r in range(3):
                    P.store("gpsimd", lambda e, a=a, g_=g_, qs=qs, r=r: e.dma_start(out=g.OMT[g_ * 192 + r * 64:g_ * 192 + (r + 1) * 64, qs], in_=OT[a][:, r, :]), bOT[a])
        P.end_stage()


def host_consts_rw():
    p = np.arange(64)[:, None]
    f = np.arange(64)[None, :]
    ls = (p > f).astype(np.float32)
    us = (p < f).astype(np.float32)
    ui = (p <= f).astype(np.float32)
    return {"c_m5": np.ascontiguousarray(np.concatenate([ls, us, us, ui, ui], 1))}


def stage_rwkv(g, mu_ap, w0_ap, w2_ap, a0_ap, a2_ap, g2_ap, kk_ap, ka_ap, rk_ap, lnw_ap, lnb_ap, c_m5):
    nc, P, S = g.nc, g.P, g.S
    NT, C = 512, 64
    NCK = NT // C
    ps, bps = g.ps, g.bps
    B = P.buf
    I64 = g.cf[0:64, 0, 0:64]
    ONES = g.cf[0:64, 1, 0:64]
    with ExitStack() as st:
        sb = lambda name, shape, dt=F32: _sb(nc, st, "rw" + name, shape, dt)
        P12 = sb("P12", [64, 12, NT + 1]); SH = sb("SH", [64, 12, NT])
        wx = sb("wx", [32, NT + 1]); ax = sb("ax", [32, NT + 1]); gx = sb("gx", [64, NT + 1])
        shw = sb("shw", [32, NT]); sha = sb("sha", [32, NT]); shg = sb("shg", [64, NT])
        AT = sb("AT", [64, 4, NT]); BT = sb("BT", [64, 4, NT]); KT = sb("KT", [64, 4, NT]); RT = sb("RT", [64, 4, NT])
        GG = sb("GG", [64, 4, NT]); RK = sb("RK", [64, 4, NT]); gC = sb("gC", [64, 4, NCK])
        OUTT = sb("OUTT", [64, 4, NT], BF16)
        t1 = sb("t1", [64, NT]); t2 = sb("t2", [64, NT]); t3 = sb("t3", [64, NT]); cs = sb("cs", [64, NT]); lnw = sb("lnw", [64, NT]); aa = sb("aa", [64, NT])
        mu12 = sb("mu12", [64, 12]); muw = sb("muw", [32, 1]); mua = sb("mua", [32, 1]); mug = sb("mug", [64, 1])
        w0s = sb("w0s", [64, 4]); a0s = sb("a0s", [64, 4]); kks = sb("kks", [64, 4]); kas = sb("kas", [64, 4]); omk = sb("omk", [64, 4]); rks = sb("rks", [64, 4])
        w2s = sb("w2s", [32, 256]); a2s = sb("a2s", [32, 256]); g2s = sb("g2s", [64, 256])
        lnwb = sb("lnwb", [64, 256]); lnbb = sb("lnbb", [64, 256])
        M5c = sb("M5c", [64, 320])
        H = sb("H", [64, 4, 64]); Hs = sb("Hs", [64, 4, 64])
        M5 = [sb("M5_%d" % h, [64, 320]) for h in range(4)]
        PQ = [[sb("PQ%d_%d" % (h, i), [64, 128]) for i in range(2)] for h in range(4)]
        TT = [sb("TT%d" % h, [64, 64]) for h in range(4)]
        VBK = [sb("VBK%d" % h, [64, 192]) for h in range(4)]
        Xs = [sb("Xs%d" % h, [64, 64]) for h in range(4)]
        Us = [sb("Us%d" % h, [64, 64]) for h in range(4)]
        cen = [sb("cen%d" % h, [64, 64]) for h in range(4)]
        yy = [sb("yy%d" % h, [64, 64]) for h in range(4)]
        sst = [sb("sst%d" % h, [64, 6]) for h in range(4)]
        junk = sb("junk", [64, 64])
        bP12, bSH, bx3, bsh3, bprm, bt1, bt2, bt3, bcs, blnw, baa, bjunk, bOUT = (B("z") for _ in range(13))
        bAT, bBT, bKT, bRT, bGG, bRK, bgC = ([B("h%d" % h) for h in range(4)] for _ in range(7))
        bH = [B("H%d" % h) for h in range(4)]
        bHs = [B("Hs%d" % h) for h in range(4)]
        bM5 = [B("M5%d" % h) for h in range(4)]
        bPQ = [[B("PQ") for i in range(2)] for h in range(4)]
        bTT, bVBK, bXs, bUs, bcen, byy, bsst = ([B("q%d" % h) for h in range(4)] for _ in range(7))

        ld = lambda dst, src, bf, small=True: P.load("sync", lambda e: e.dma_start(out=dst, in_=src, allow_slow_non_contiguous=small), bf)
        ld(mu12[:], mu_ap[0:768].rearrange("(a p) -> p a", p=64), bprm)
        ld(muw[:], mu_ap[768:800].rearrange("(p o) -> p o", o=1), bprm)
        ld(mua[:], mu_ap[800:832].rearrange("(p o) -> p o", o=1), bprm)
        ld(mug[:], mu_ap[832:896].rearrange("(p o) -> p o", o=1), bprm)
        ld(w0s[:], w0_ap.rearrange("(h p) -> p h", p=64), bprm)
        ld(a0s[:], a0_ap.rearrange("(h p) -> p h", p=64), bprm)
        ld(kks[:], kk_ap.rearrange("(h p) -> p h", p=64), bprm)
        ld(kas[:], ka_ap.rearrange("(h p) -> p h", p=64), bprm)
        ld(rks[:], rk_ap.rearrange("h p -> p h"), bprm)
        ld(w2s[:], w2_ap[:, :], bprm)
        ld(a2s[:], a2_ap[:, :], bprm)
        ld(g2s[:], g2_ap[:, :], bprm)
        ld(lnwb[:], lnw_ap.partition_broadcast(64), bprm)
        ld(lnbb[:], lnb_ap.partition_broadcast(64), bprm)
        ld(M5c[:], c_m5[:, :], bprm)
        P.op("vector", lambda e: e.tensor_scalar(omk[:], kas[:], -1.0, 1.0, ALU.mult, ALU.add), reads=[bprm], writes=[bprm])
        for h in range(4):
            P.op("vector", lambda e, h=h: e.memset(H[:, h, :], 0.0), writes=[bH[h]])
        rwv = g.RW[0:768, :].rearrange("(a p) t -> p a t", p=64)

        for tt in range(S // NT):
            t0 = tt * NT
            if tt == 0:
                P.op("vector", lambda e: e.memset(P12[:, :, 0:1], 0.0), writes=[bP12])
                P.op("vector", lambda e: e.memset(wx[:, 0:1], 0.0), writes=[bx3])
                P.op("vector", lambda e: e.memset(ax[:, 0:1], 0.0), writes=[bx3])
                P.op("vector", lambda e: e.memset(gx[:, 0:1], 0.0), writes=[bx3])
                lo, c0 = 1, 0
            else:
                lo, c0 = 0, t0 - 1
            for i in range(12):
                P.load("sync" if i % 2 == 0 else "gpsimd", lambda e, lo=lo, c0=c0, t0=t0, i=i: e.dma_start(out=P12[:, i, lo:NT + 1], in_=g.RW[i * 64:(i + 1) * 64, c0:t0 + NT]), bP12)
            P.load("gpsimd", lambda e, lo=lo, c0=c0, t0=t0: e.dma_start(out=wx[:, lo:NT + 1], in_=g.RW[768:800, c0:t0 + NT]), bx3)
            P.load("gpsimd", lambda e, lo=lo, c0=c0, t0=t0: e.dma_start(out=ax[:, lo:NT + 1], in_=g.RW[800:832, c0:t0 + NT]), bx3)
            P.load("gpsimd", lambda e, lo=lo, c0=c0, t0=t0: e.dma_start(out=gx[:, lo:NT + 1], in_=g.RW[832:896, c0:t0 + NT]), bx3)
            for i in range(12):
                P.op("vector", lambda e, i=i: e.tensor_tensor(SH[:, i, :], P12[:, i, 0:NT], P12[:, i, 1:NT + 1], ALU.subtract), reads=[bP12], writes=[bSH])
                P.op("vector", lambda e, i=i: e.scalar_tensor_tensor(SH[:, i, :], SH[:, i, :], mu12[:, i:i + 1], P12[:, i, 1:NT + 1], ALU.mult, ALU.add),
                     reads=[bP12, bSH, bprm], writes=[bSH])
            for (xx, ss, mm) in ((wx, shw, muw), (ax, sha, mua), (gx, shg, mug)):
                P.op("vector", lambda e, xx=xx, ss=ss: e.tensor_tensor(ss[:], xx[:, 0:NT], xx[:, 1:NT + 1], ALU.subtract), reads=[bx3], writes=[bsh3])
                P.op("vector", lambda e, xx=xx, ss=ss, mm=mm: e.scalar_tensor_tensor(ss[:], ss[:], mm[:, 0:1], xx[:, 1:NT + 1], ALU.mult, ALU.add),
                     reads=[bx3, bsh3, bprm], writes=[bsh3])
            P.op("scalar", lambda e: e.activation(out=shw[:], in_=shw[:], func=AF.Tanh), reads=[bsh3], writes=[bsh3])
            P.op("scalar", lambda e: e.activation(out=shg[:], in_=shg[:], func=AF.Sigmoid), reads=[bsh3], writes=[bsh3])
            for h in range(4):
                hs_ = slice(h * 64, (h + 1) * 64)
                r_, k_, v_ = SH[:, h, :], SH[:, 4 + h, :], SH[:, 8 + h, :]
                P.op("tensor", lambda e, hs_=hs_: e.matmul(ps[0][0:64, 0:NT], w2s[:, hs_], shw[:], start=True, stop=True), reads=[bprm, bsh3], writes=[bps[0]])
                P.op("tensor", lambda e, hs_=hs_: e.matmul(ps[1][0:64, 0:NT], a2s[:, hs_], sha[:], start=True, stop=True), reads=[bprm, bsh3], writes=[bps[1]])
                P.op("tensor", lambda e, hs_=hs_: e.matmul(ps[2][0:64, 0:NT], g2s[:, hs_], shg[:], start=True, stop=True), reads=[bprm, bsh3], writes=[bps[2]])
                P.op("scalar", lambda e, h=h: e.activation(out=lnw[:], in_=ps[0][0:64, 0:NT], func=AF.Sigmoid, bias=w0s[:, h:h + 1]), reads=[bps[0], bprm], writes=[blnw])
                P.op("vector", lambda e: e.tensor_scalar(lnw[:], lnw[:], -0.606531, None, ALU.mult), reads=[blnw], writes=[blnw])
                P.op("scalar", lambda e, h=h: e.activation(out=aa[:], in_=ps[1][0:64, 0:NT], func=AF.Sigmoid, bias=a0s[:, h:h + 1]), reads=[bps[1], bprm], writes=[baa])
                P.op("scalar", lambda e, h=h: e.activation(out=GG[:, h, :], in_=ps[2][0:64, 0:NT], func=AF.Copy), reads=[bps[2]], writes=[bGG[h]])
                P.op("vector", lambda e, h=h, k_=k_: e.tensor_scalar(t1[:], k_, kks[:, h:h + 1], None, ALU.mult), reads=[bSH, bprm], writes=[bt1])
                P.op("scalar", lambda e: e.activation(out=t2[:], in_=t1[:], func=AF.Square), reads=[bt1], writes=[bt2])
                P.op("tensor", lambda e: e.matmul(ps[3][0:64, 0:NT], ONES, t2[:], start=True, stop=True), reads=[g.bcf, bt2], writes=[bps[3]])
                P.op("scalar", lambda e: e.activation(out=t2[:], in_=ps[3][0:64, 0:NT], func=AF.Sqrt), reads=[bps[3]], writes=[bt2])
                P.op("vector", lambda e: e.tensor_scalar(t2[:], t2[:], 1e-12, None, ALU.max), reads=[bt2], writes=[bt2])
                P.op("vector", lambda e: e.reciprocal(t2[:], t2[:]), reads=[bt2], writes=[bt2])
                P.op("vector", lambda e: e.tensor_tensor(t1[:], t1[:], t2[:], ALU.mult), reads=[bt1, bt2], writes=[bt1])
                P.op("vector", lambda e, h=h: e.tensor_scalar(t3[:], aa[:], kas[:, h:h + 1], omk[:, h:h + 1], ALU.mult, ALU.add), reads=[baa, bprm], writes=[bt3])
                P.op("vector", lambda e, k_=k_: e.tensor_tensor(t3[:], t3[:], k_, ALU.mult), reads=[bt3, bSH], writes=[bt3])
                P.op("vector", lambda e, h=h, r_=r_: e.scalar_tensor_tensor(RK[:, h, :], r_, rks[:, h:h + 1], t3[:], ALU.mult, ALU.mult),
                     reads=[bSH, bprm, bt3], writes=[bRK[h]])
                for c in range(NCK):
                    cc = slice(c * C, (c + 1) * C)
                    P.op("vector", lambda e, cc=cc: e.tensor_tensor_scan(cs[:, cc], g.cf[0:64, 1, 0:C], lnw[:, cc], 0.0, ALU.mult, ALU.add),
                         reads=[blnw, g.bcf], writes=[bcs])
                P.op("scalar", lambda e, h=h, r_=r_: e.activation(out=t2[:], in_=cs[:], func=AF.Exp), reads=[bcs], writes=[bt2])
                P.op("vector", lambda e, h=h, r_=r_: e.tensor_tensor(RT[:, h, :], r_, t2[:], ALU.mult), reads=[bSH, bt2], writes=[bRT[h]])
                P.op("vector", lambda e, h=h: e.tensor_copy(gC[:, h, :], t2[:, C - 1:NT:C]), reads=[bt2], writes=[bgC[h]])
                P.op("scalar", lambda e: e.activation(out=t2[:], in_=cs[:], func=AF.Exp, scale=-1.0), reads=[bcs], writes=[bt2])
                P.op("vector", lambda e, h=h: e.tensor_tensor(KT[:, h, :], t3[:], t2[:], ALU.mult), reads=[bt3, bt2], writes=[bKT[h]])
                P.op("vector", lambda e: e.tensor_tensor(t3[:], t1[:], aa[:], ALU.mult), reads=[bt1, baa], writes=[bt3])
                P.op("vector", lambda e, h=h: e.tensor_tensor(BT[:, h, :], t3[:], t2[:], ALU.mult), reads=[bt3, bt2], writes=[bBT[h]])
                P.op("vector", lambda e: e.tensor_tensor(t2[:], cs[:], lnw[:], ALU.subtract), reads=[bcs, blnw], writes=[bt2])
                P.op("scalar", lambda e: e.activation(out=t2[:], in_=t2[:], func=AF.Exp), reads=[bt2], writes=[bt2])
                P.op("vector", lambda e, h=h: e.scalar_tensor_tensor(AT[:, h, :], t1[:], -1.0, t2[:], ALU.mult, ALU.mult), reads=[bt1, bt2], writes=[bAT[h]])
            HB = lambda h: 4 + h
            for c in range(NCK):
                cc = slice(c * C, (c + 1) * C)
                for h in range(4):
                    A_, B_, K_, R_ = AT[:, h, cc], BT[:, h, cc], KT[:, h, cc], RT[:, h, cc]
                    rd = [bAT[h], bBT[h], bKT[h], bRT[h]]
                    for i, (l_, r2) in enumerate(((A_, B_), (B_, A_), (K_, A_), (B_, R_), (K_, R_))):
                        P.op("tensor", lambda e, h=h, i=i, l_=l_, r2=r2: e.matmul(ps[h][0:64, i * 64:(i + 1) * 64], l_, r2, start=(i == 0), stop=True, skip_group_check=True),
                             reads=rd, writes=[bps[h]], inc=(i == 4))
                    P.op("vector", lambda e, h=h: e.tensor_tensor(M5[h][:], ps[h][0:64, 0:320], M5c[:], ALU.mult), reads=[bps[h], bprm], writes=[bM5[h]])
                    for i, src in enumerate((SH[:, 8 + h, cc], B_, K_)):
                        P.op("tensor", lambda e, h=h, i=i, src=src: e.transpose(ps[HB(h)][0:64, 256 + i * 64:256 + (i + 1) * 64], src, I64),
                             reads=[bSH, bBT[h], bKT[h], g.bcf], writes=[bps[HB(h)]], inc=(i == 2))
                    P.op("scalar", lambda e, h=h: e.activation(out=VBK[h][:], in_=ps[HB(h)][0:64, 256:448], func=AF.Copy), reads=[bps[HB(h)]], writes=[bVBK[h]])
                    P.op("vector", lambda e, h=h: e.tensor_tensor(TT[h][:], M5[h][:, 64:128], I64, ALU.add), reads=[bM5[h], g.bcf], writes=[bTT[h]])
                    P.op("vector", lambda e, h=h, c=c: e.tensor_scalar(Hs[:, h, :], H[:, h, :], gC[:, h, c:c + 1], None, ALU.mult), reads=[bH[h], bgC[h]], writes=[bHs[h]])
                cur = [(M5[h], bM5[h]) for h in range(4)]
                for it in range(5):
                    for h in range(4):
                        ct, cb_ = cur[h]
                        P.op("tensor", lambda e, h=h, ct=ct: e.matmul(ps[h][0:64, 320:384], ct[:, 64:128], ct[:, 0:64], start=True, stop=True, skip_group_check=True),
                             reads=[cb_], writes=[bps[h]], inc=(it == 4))
                        if it < 4:
                            P.op("tensor", lambda e, h=h, ct=ct: e.matmul(ps[h][0:64, 384:448], ct[:, 0:64], ct[:, 64:128], start=False, stop=True, skip_group_check=True),
                                 reads=[cb_], writes=[bps[h]])
                    for h in range(4):
                        nt_, nb_ = PQ[h][it % 2], bPQ[h][it % 2]
                        P.op("scalar", lambda e, h=h, nt_=nt_: e.activation(out=nt_[:], in_=ps[h][0:64, 320:448], func=AF.Copy), reads=[bps[h]], writes=[nb_])
                        cur[h] = (nt_, nb_)
                    for h in range(4):
                        ct, cb_ = cur[h]
                        P.op("tensor", lambda e, h=h, ct=ct: e.matmul(ps[h][0:64, 448:512], ct[:, 0:64], TT[h][:], start=True, stop=True, skip_group_check=True),
                             reads=[cb_, bTT[h]], writes=[bps[h]])
                    for h in range(4):
                        P.op("vector", lambda e, h=h: e.tensor_tensor(TT[h][:], ps[h][0:64, 448:512], TT[h][:], ALU.add), reads=[bps[h], bTT[h]], writes=[bTT[h]])
                for h in range(4):
                    A_, R_ = AT[:, h, cc], RT[:, h, cc]
                    P.op("tensor", lambda e, h=h, A_=A_: e.matmul(ps[HB(h)][0:64, 0:64], A_, H[:, h, :], start=True, stop=False, skip_group_check=True),
                         reads=[bAT[h], bH[h]], writes=[bps[HB(h)]], inc=False)
                    P.op("tensor", lambda e, h=h: e.matmul(ps[HB(h)][0:64, 0:64], M5[h][:, 128:192], VBK[h][:, 0:64], start=False, stop=True, skip_group_check=True),
                         reads=[bM5[h], bVBK[h]], writes=[bps[HB(h)]])
                for h in range(4):
                    P.op("vector", lambda e, h=h: e.tensor_copy(Xs[h][:], ps[HB(h)][0:64, 0:64]), reads=[bps[HB(h)]], writes=[bXs[h]])
                for h in range(4):
                    P.op("tensor", lambda e, h=h: e.matmul(ps[HB(h)][0:64, 64:128], TT[h][:], Xs[h][:], start=True, stop=True, skip_group_check=True),
                         reads=[bTT[h], bXs[h]], writes=[bps[HB(h)]])
                for h in range(4):
                    P.op("scalar", lambda e, h=h: e.activation(out=Us[h][:], in_=ps[HB(h)][0:64, 64:128], func=AF.Copy), reads=[bps[HB(h)]], writes=[bUs[h]])
                for h in range(4):
                    R_ = RT[:, h, cc]
                    P.op("tensor", lambda e, h=h, R_=R_: e.matmul(ps[HB(h)][0:64, 128:192], R_, H[:, h, :], start=True, stop=False, skip_group_check=True),
                         reads=[bRT[h], bH[h]], writes=[bps[HB(h)]], inc=False)
                    P.op("tensor", lambda e, h=h: e.matmul(ps[HB(h)][0:64, 128:192], M5[h][:, 192:256], Us[h][:], start=False, stop=False, skip_group_check=True),
                         reads=[bM5[h], bUs[h]], writes=[bps[HB(h)]], inc=False)
                    P.op("tensor", lambda e, h=h: e.matmul(ps[HB(h)][0:64, 128:192], M5[h][:, 256:320], VBK[h][:, 0:64], start=False, stop=True, skip_group_check=True),
                         reads=[bM5[h], bVBK[h]], writes=[bps[HB(h)]], inc=False)
                    P.op("tensor", lambda e, h=h, cc=cc: e.matmul(ps[HB(h)][0:64, 192:193], RK[:, h, cc], g.cf[0:64, 1, 0:1], start=False, stop=True, skip_group_check=True),
                         reads=[bRK[h], g.bcf], writes=[bps[HB(h)]], inc=False)
                    P.op("tensor", lambda e, h=h: e.matmul(ps[HB(h)][0:64, 194:258], VBK[h][:, 64:128], Us[h][:], start=False, stop=False, skip_group_check=True),
                         reads=[bVBK[h], bUs[h]], writes=[bps[HB(h)]], inc=False)
                    P.op("tensor", lambda e, h=h: e.matmul(ps[HB(h)][0:64, 194:258], VBK[h][:, 128:192], VBK[h][:, 0:64], start=False, stop=True, skip_group_check=True),
                         reads=[bVBK[h]], writes=[bps[HB(h)]])
                for h in range(4):
                    pO = ps[HB(h)][0:64, 128:192]
                    P.op("vector", lambda e, h=h, c=c: e.scalar_tensor_tensor(H[:, h, :], ps[HB(h)][0:64, 194:258], gC[:, h, c:c + 1], Hs[:, h, :], ALU.mult, ALU.add),
                         reads=[bps[HB(h)], bgC[h], bHs[h]], writes=[bH[h]])
                    P.op("scalar", lambda e, h=h, pO=pO: e.activation(out=junk[:], in_=pO, func=AF.Identity, accum_out=sst[h][:, 0:1]), reads=[bps[HB(h)]], writes=[bjunk, bsst[h]])
                    P.op("vector", lambda e, h=h: e.tensor_scalar(sst[h][:, 1:2], sst[h][:, 0:1], 1.0 / 64, None, ALU.mult), reads=[bsst[h]], writes=[bsst[h]])
                    P.op("vector", lambda e, h=h, pO=pO: e.tensor_scalar(cen[h][:], pO, sst[h][:, 1:2], None, ALU.subtract), reads=[bps[HB(h)], bsst[h]], writes=[bcen[h]])
                    P.op("scalar", lambda e, h=h: e.activation(out=junk[:], in_=cen[h][:], func=AF.Square, accum_out=sst[h][:, 2:3]), reads=[bcen[h]], writes=[bjunk, bsst[h]])
                    P.op("scalar", lambda e, h=h: e.activation(out=sst[h][:, 3:4], in_=sst[h][:, 2:3], func=AF.Sqrt, scale=1.0 / 64, bias=64e-5), reads=[bsst[h]], writes=[bsst[h]])
                    P.op("vector", lambda e, h=h: e.reciprocal(sst[h][:, 4:5], sst[h][:, 3:4]), reads=[bsst[h]], writes=[bsst[h]])
                    P.op("vector", lambda e, h=h: e.scalar_tensor_tensor(yy[h][:], cen[h][:], sst[h][:, 4:5], lnwb[:, h * 64:(h + 1) * 64], ALU.mult, ALU.mult),
                         reads=[bcen[h], bsst[h], bprm], writes=[byy[h]])
                    P.op("vector", lambda e, h=h: e.tensor_tensor(yy[h][:], yy[h][:], lnbb[:, h * 64:(h + 1) * 64], ALU.add), reads=[byy[h], bprm], writes=[byy[h]])
                    P.op("vector", lambda e, h=h: e.tensor_copy(sst[h][:, 5:6], ps[HB(h)][0:64, 192:193]), reads=[bps[HB(h)]], writes=[bsst[h]])
                    P.op("vector", lambda e, h=h: e.scalar_tensor_tensor(yy[h][:], VBK[h][:, 0:64], sst[h][:, 5:6], yy[h][:], ALU.mult, ALU.add),
                         reads=[bVBK[h], bsst[h], byy[h]], writes=[byy[h]])
                    P.op("tensor", lambda e, h=h: e.transpose(ps[HB(h)][0:64, 448:512], yy[h][:], I64), reads=[byy[h], g.bcf], writes=[bps[HB(h)]])
                    P.op("vector", lambda e, h=h, cc=cc: e.tensor_tensor(OUTT[:, h, cc], ps[HB(h)][0:64, 448:512], GG[:, h, cc], ALU.mult), reads=[bps[HB(h)], bGG[h]], writes=[bOUT])
            for h in range(4):
                P.store("gpsimd", lambda e, t0=t0, h=h: e.dma_start(out=g.OMT[384 + h * 64:384 + (h + 1) * 64, t0:t0 + NT], in_=OUTT[:, h, :]), bOUT)
        P.end_stage()


_PARAM_SHAPES = {
    "attn_norm": [DEPTH, 1024], "w_in": [DEPTH, 1024, N_IN], "nsa_cmp_pos": [DEPTH, 2, 32, 64], "nsa_cmp_w1": [DEPTH, 2, 2048, 256],
    "nsa_cmp_w2": [DEPTH, 2, 256, 64], "nsa_out_gain": [DEPTH, 384], "rw_mu": [DEPTH, 896], "rw_w0": [DEPTH, 256], "rw_w2": [DEPTH, 32, 256],
    "rw_a0": [DEPTH, 256], "rw_a2": [DEPTH, 32, 256], "rw_g2": [DEPTH, 64, 256], "rw_k_k": [DEPTH, 256], "rw_k_a": [DEPTH, 256],
    "rw_r_k": [DEPTH, 4, 64], "rw_lnx_w": [DEPTH, 256], "rw_lnx_b": [DEPTH, 256], "moba_out_gain": [DEPTH, 384], "w_out": [DEPTH, 1024, 1024],
    "ffn_norm": [DEPTH, 1024], "ffn_w_in": [DEPTH, 1024, 2 * D_FF], "ffn_conv_w": [DEPTH, 3, D_FF], "ffn_conv_b": [DEPTH, D_FF],
    "ffn_w_out": [DEPTH, D_FF, 1024], "final_norm": [1024],
}


def all_host_consts(S):
    c = {}
    c.update(host_consts())
    c.update(host_consts_s(S))
    c.update(host_consts_nsa(S))
    c.update(host_consts_rw())
    return c


def build_full(S=SEQ, depth=DEPTH, stages="pnrm3f", debug=False):
    nc = bass.Bass("TRN2", target_bir_lowering=False)
    inp = lambda name, shape, dt=F32: nc.dram_tensor(name, list(shape), dt, kind="ExternalInput").ap()
    xT = inp("xT", [D_MODEL, S])
    A = {k: inp(k, v) for k, v in _PARAM_SHAPES.items()}
    hc = all_host_consts(S)
    CA = {k: inp(k, v.shape, F32 if v.dtype == np.float32 else BF16) for k, v in hc.items() if k not in ("c_cb", "c_cf")}
    outT = nc.dram_tensor("outT", [D_MODEL, S], F32, kind="ExternalOutput").ap()
    with ExitStack() as es:
        g = setup_globals(nc, es, S, debug=debug)
        load_consts(g)
        for l in range(depth):
            x_src = xT if l == 0 else g.X
            if "p" in stages:
                stage_p1(g, x_src, A["attn_norm"][l], A["w_in"][l])
            if "n" in stages:
              stage_nsa(g, A["nsa_out_gain"][l], A["nsa_cmp_pos"][l], A["nsa_cmp_w1"][l], A["nsa_cmp_w2"][l],
                      CA["c_ohs"], CA["c_cm"], CA["c_cmap"], CA["c_fbw"])
            if "r" in stages:
              stage_rwkv(g, A["rw_mu"][l], A["rw_w0"][l], A["rw_w2"][l], A["rw_a0"][l], A["rw_a2"][l], A["rw_g2"][l], A["rw_k_k"][l],
                       A["rw_k_a"][l], A["rw_r_k"][l], A["rw_lnx_w"][l], A["rw_lnx_b"][l], CA["c_m5"])
            if "m" in stages:
                stage_moba(g, A["moba_out_gain"][l], CA["c_ohm"])
            if "3" in stages:
                stage_p3a(g, x_src, A["w_out"][l])
                stage_p3b(g, A["ffn_norm"][l], A["ffn_w_in"][l], A["ffn_conv_w"][l], A["ffn_conv_b"][l], A["ffn_w_out"][l])
        if "f" in stages:
            stage_final(g, A["final_norm"], outT)
    return nc, g


def kernel(**inputs):
    x = np.asarray(inputs["x"], np.float32)
    Bn, S, D = x.shape
    nc, g = build_full(S)
    hc = all_host_consts(S)
    maps = []
    for b in range(Bn):
        m = {"xT": np.ascontiguousarray(x[b].T)}
        for k in _PARAM_SHAPES:
            m[k] = np.ascontiguousarray(np.asarray(inputs[k], np.float32))
        m.update(hc)
        maps.append(m)
    res = run_bass_kernel_spmd(nc, maps, core_ids=list(range(Bn)))
    out = np.stack([np.ascontiguousarray(np.asarray(res.results[b]["outT"], np.float32).T) for b in range(Bn)], 0)
    return out
```
